# Optimizing a Trainium2 kernel written in Bass

```python
import math
import jax
import jax.numpy as jnp
from jax import lax
import numpy as np

D_MODEL = 1024
BATCH = 8
SEQ = 4096
DEPTH = 1

GLA_HEADS = 4
GLA_DK = D_MODEL // (2 * GLA_HEADS)
GLA_DV = D_MODEL // GLA_HEADS
GLA_GATE_RANK = 16
GLA_TAU = 16.0
GLA_CHUNK = 64
DIFF_HEADS = 8
DIFF_DH = D_MODEL // (2 * DIFF_HEADS)
ROPE_THETA = 10000.0
Q_BLOCK = 128
N_GROUPS = 4
EXPERTS_PER_GROUP = 8
N_EXPERTS = N_GROUPS * EXPERTS_PER_GROUP
TOP_K_IN_GROUP = 2
EXPERT_FF = 512
MOE_BLOCK = 128
LN_EPS = 1e-5
RMS_EPS = 1e-6
IN_SPLITS = (GLA_HEADS * GLA_DK, GLA_HEADS * GLA_DK, GLA_HEADS * GLA_DV, GLA_HEADS * GLA_DV, GLA_GATE_RANK,
             DIFF_HEADS * 2 * DIFF_DH, DIFF_HEADS * 2 * DIFF_DH, DIFF_HEADS * 2 * DIFF_DH, 2 * D_MODEL)
IN_COLS = sum(IN_SPLITS)

kernel_name = "hybrid_gla_diffattn_hmoe_deepnorm_adaln"


def _split_points():
    pts, acc = [], 0
    for s in IN_SPLITS[:-1]:
        acc += s
        pts.append(acc)
    return pts


def _layer_norm(x):
    xf = x.astype(jnp.float32)
    mu = jnp.mean(xf, -1, keepdims=True)
    var = jnp.mean(jnp.square(xf - mu), -1, keepdims=True)
    return (xf - mu) * lax.rsqrt(var + LN_EPS)


def _rms_norm(x, w):
    xf = x.astype(jnp.float32)
    y = xf * lax.rsqrt(jnp.mean(jnp.square(xf), -1, keepdims=True) + RMS_EPS) * w.astype(jnp.float32)
    return y


def _rope(t, pos):
    half = t.shape[-1] // 2
    inv = ROPE_THETA ** (-jnp.arange(half, dtype=jnp.float32) / half)
    ang = pos.astype(jnp.float32)[:, :, None, None, None] * inv
    cos, sin = jnp.cos(ang), jnp.sin(ang)
    tf = t.astype(jnp.float32)
    t1, t2 = tf[..., :half], tf[..., half:]
    return jnp.concatenate([t1 * cos - t2 * sin, t2 * cos + t1 * sin], -1).astype(t.dtype)


def _gla_chunked(q, k, v, log_g):
    B, H, S, dk = q.shape
    dv = v.shape[-1]
    C = GLA_CHUNK
    N = S // C
    f32 = jnp.float32
    q = q.astype(f32).reshape(B, H, N, C, dk) * (dk ** -0.5)
    k = k.astype(f32).reshape(B, H, N, C, dk)
    v = v.astype(f32).reshape(B, H, N, C, dv)
    b = jnp.cumsum(log_g.astype(f32).reshape(B, H, N, C, dk), axis=3)
    b_last = b[:, :, :, -1:, :]
    q_t = q * jnp.exp(b)
    k_t = k * jnp.exp(-b)
    k_d = k * jnp.exp(b_last - b)
    causal = jnp.tril(jnp.ones((C, C), dtype=bool))
    attn = jnp.where(causal, jnp.einsum('bhnid,bhnjd->bhnij', q_t, k_t), 0.0)
    o_intra = jnp.einsum('bhnij,bhnjv->bhniv', attn, v)
    decay = jnp.exp(b_last[:, :, :, 0, :])

    def step(state, inp):
        qn, kn, vn, dn = inp
        o = jnp.einsum('bhid,bhdv->bhiv', qn, state)
        state = state * dn[..., None] + jnp.einsum('bhjd,bhjv->bhdv', kn, vn)
        return state, o

    xs = (jnp.moveaxis(q_t, 2, 0), jnp.moveaxis(k_d, 2, 0), jnp.moveaxis(v, 2, 0), jnp.moveaxis(decay, 2, 0))
    _, o_inter = lax.scan(step, jnp.zeros((B, H, dk, dv), f32), xs)
    o = o_intra + jnp.moveaxis(o_inter, 0, 2)
    return o.reshape(B, H, S, dv)


def _gla_branch(q_in, k_in, v_in, og_in, glr, w_g2, b_g2, norm_w):
    B, S, _ = q_in.shape
    log_g = jax.nn.log_sigmoid((glr @ w_g2 + b_g2).astype(jnp.float32)) / GLA_TAU

    def heads(t, d):
        return t.reshape(B, S, GLA_HEADS, d).transpose(0, 2, 1, 3)

    o = _gla_chunked(heads(q_in, GLA_DK), heads(k_in, GLA_DK), heads(v_in, GLA_DV), heads(log_g, GLA_DK))
    o = o.transpose(0, 2, 1, 3)
    o = _rms_norm(o, norm_w) * jax.nn.silu(og_in.astype(jnp.float32)).reshape(B, S, GLA_HEADS, GLA_DV)
    return o.reshape(B, S, GLA_HEADS * GLA_DV).astype(q_in.dtype)


def _diff_branch(q_in, k_in, v_in, pos, lq1, lk1, lq2, lk2, norm_w, lam_init):
    B, S, _ = q_in.shape
    H, dh = DIFF_HEADS, DIFF_DH
    nb = S // Q_BLOCK
    q = _rope(q_in.reshape(B, S, H, 2, dh), pos) * (dh ** -0.5)
    k = _rope(k_in.reshape(B, S, H, 2, dh), pos)
    q = jnp.moveaxis(q.transpose(0, 2, 3, 1, 4).reshape(B, H, 2, nb, Q_BLOCK, dh), 3, 0)
    k = k.transpose(0, 2, 3, 1, 4)
    v = v_in.reshape(B, S, H, 2 * dh).transpose(0, 2, 1, 3)
    f32 = jnp.float32
    lam = (jnp.exp(jnp.sum(lq1.astype(f32) * lk1.astype(f32)))
           - jnp.exp(jnp.sum(lq2.astype(f32) * lk2.astype(f32))) + lam_init)
    key_pos = jnp.arange(S)

    def block(args):
        qb, bi = args
        s = jnp.einsum('bhcqd,bhckd->bhcqk', qb, k, preferred_element_type=f32)
        q_pos = bi * Q_BLOCK + jnp.arange(Q_BLOCK)
        s = jnp.where(key_pos[None, :] <= q_pos[:, None], s, -jnp.inf)
        p = jax.nn.softmax(s, axis=-1)
        a = p[:, :, 0] - lam * p[:, :, 1]
        return jnp.einsum('bhqk,bhkd->bhqd', a.astype(v.dtype), v)

    o = lax.map(block, (q, jnp.arange(nb)))
    o = jnp.moveaxis(o, 0, 2).reshape(B, H, S, 2 * dh).transpose(0, 2, 1, 3)
    o = _rms_norm(o, norm_w) * (1.0 - lam_init)
    return o.reshape(B, S, H * 2 * dh).astype(q_in.dtype)


def _hier_moe(u, w_rg, b_rg, w_re, b_re, w_gate, w_up, w_down):
    B, S, D = u.shape
    T = B * S
    xf = u.reshape(T, D)
    f32 = jnp.float32
    p_group = jax.nn.softmax((xf @ w_rg + b_rg).astype(f32), axis=-1)
    w_grp, g_idx = lax.top_k(p_group, 1)
    e_logits = (xf @ w_re + b_re).astype(f32).reshape(T, N_GROUPS, EXPERTS_PER_GROUP)
    e_sel = jnp.take_along_axis(e_logits, g_idx[:, :, None], axis=1)[:, 0]
    v2, i2 = lax.top_k(e_sel, TOP_K_IN_GROUP)
    comb = w_grp * jax.nn.softmax(v2, axis=-1)
    eid = g_idx * EXPERTS_PER_GROUP + i2
    tk = T * TOP_K_IN_GROUP
    flat_e = eid.reshape(tk).astype(jnp.int32)
    flat_t = jnp.repeat(jnp.arange(T, dtype=jnp.int32), TOP_K_IN_GROUP)
    flat_w = comb.reshape(tk).astype(u.dtype)
    order = jnp.argsort(flat_e, stable=True)
    sorted_e = flat_e[order]
    counts = jnp.bincount(flat_e, length=N_EXPERTS)
    start = jnp.cumsum(counts) - counts
    pcounts = ((counts + MOE_BLOCK - 1) // MOE_BLOCK) * MOE_BLOCK
    pend = jnp.cumsum(pcounts)
    pstart = pend - pcounts
    dest = pstart[sorted_e] + (jnp.arange(tk) - start[sorted_e])
    P = ((tk + N_EXPERTS * (MOE_BLOCK - 1) + MOE_BLOCK - 1) // MOE_BLOCK) * MOE_BLOCK
    nblk = P // MOE_BLOCK
    row_token = jnp.zeros((P,), jnp.int32).at[dest].set(flat_t[order])
    row_w = jnp.zeros((P,), u.dtype).at[dest].set(flat_w[order])
    block_expert = jnp.clip(jnp.searchsorted(pend, jnp.arange(nblk) * MOE_BLOCK, side='right'),
                            0, N_EXPERTS - 1).astype(jnp.int32)
    xs = xf[row_token].reshape(nblk, MOE_BLOCK, D)

    def expert_block(args):
        xb, e = args
        h = jax.nn.silu(xb @ w_gate[e]) * (xb @ w_up[e])
        return h @ w_down[e]

    rows = lax.map(expert_block, (xs, block_expert)).reshape(P, D)
    y = jax.ops.segment_sum(rows * row_w[:, None], row_token, num_segments=T)
    return y.reshape(B, S, D)


def setup_inputs(seed: int = 0) -> dict:
    key = jax.random.key(seed)
    ks = jax.random.split(key, 26)
    f32 = jnp.float32
    beta = (8.0 * DEPTH) ** -0.25
    Ldim = DEPTH

    def nrm(k, shape, s):
        return jax.random.normal(k, shape, f32) * s

    return {
        "x": nrm(ks[0], (BATCH, SEQ, D_MODEL), 1.0),
        "c": nrm(ks[1], (BATCH, D_MODEL), 1.0),
        "positions": (jnp.arange(SEQ, dtype=jnp.int32)[None, :]
                      + jax.random.randint(ks[2], (BATCH, 1), 0, SEQ, dtype=jnp.int32)),
        "w_ada": nrm(ks[3], (Ldim, D_MODEL, 6 * D_MODEL), 0.5 * D_MODEL ** -0.5),
        "b_ada": nrm(ks[4], (Ldim, 6 * D_MODEL), 0.01),
        "w_in": nrm(ks[5], (Ldim, D_MODEL, IN_COLS), D_MODEL ** -0.5),
        "w_gla_gate2": nrm(ks[6], (Ldim, GLA_GATE_RANK, GLA_HEADS * GLA_DK), GLA_GATE_RANK ** -0.5),
        "b_gla_gate2": nrm(ks[7], (Ldim, GLA_HEADS * GLA_DK), 0.01),
        "gla_norm_w": 1.0 + nrm(ks[8], (Ldim, GLA_DV), 0.02),
        "diff_lambda_q1": nrm(ks[9], (Ldim, DIFF_DH), 0.1),
        "diff_lambda_k1": nrm(ks[10], (Ldim, DIFF_DH), 0.1),
        "diff_lambda_q2": nrm(ks[11], (Ldim, DIFF_DH), 0.1),
        "diff_lambda_k2": nrm(ks[12], (Ldim, DIFF_DH), 0.1),
        "diff_norm_w": 1.0 + nrm(ks[13], (Ldim, 2 * DIFF_DH), 0.02),
        "w_out": nrm(ks[14], (Ldim, D_MODEL, D_MODEL), beta * D_MODEL ** -0.5),
        "ln1_w": 1.0 + nrm(ks[15], (Ldim, D_MODEL), 0.02),
        "ln1_b": nrm(ks[16], (Ldim, D_MODEL), 0.02),
        "w_router_group": nrm(ks[17], (Ldim, D_MODEL, N_GROUPS), D_MODEL ** -0.5),
        "b_router_group": nrm(ks[18], (Ldim, N_GROUPS), 0.01),
        "w_router_expert": nrm(ks[19], (Ldim, D_MODEL, N_EXPERTS), D_MODEL ** -0.5),
        "b_router_expert": nrm(ks[20], (Ldim, N_EXPERTS), 0.01),
        "w_exp_gate": nrm(ks[21], (Ldim, N_EXPERTS, D_MODEL, EXPERT_FF), D_MODEL ** -0.5),
        "w_exp_up": nrm(ks[22], (Ldim, N_EXPERTS, D_MODEL, EXPERT_FF), D_MODEL ** -0.5),
        "w_exp_down": nrm(ks[23], (Ldim, N_EXPERTS, EXPERT_FF, D_MODEL), beta * EXPERT_FF ** -0.5),
        "ln2_w": 1.0 + nrm(ks[24], (Ldim, D_MODEL), 0.02),
        "ln2_b": nrm(ks[25], (Ldim, D_MODEL), 0.02),
    }


def reference(x, c, positions, w_ada, b_ada, w_in, w_gla_gate2, b_gla_gate2, gla_norm_w,
              diff_lambda_q1, diff_lambda_k1, diff_lambda_q2, diff_lambda_k2, diff_norm_w, w_out,
              ln1_w, ln1_b, w_router_group, b_router_group, w_router_expert, b_router_expert,
              w_exp_gate, w_exp_up, w_exp_down, ln2_w, ln2_b):
    alpha = (2.0 * DEPTH) ** 0.25
    dt = x.dtype
    for l in range(DEPTH):
        lam_init = 0.8 - 0.6 * math.exp(-0.3 * l)
        ada = jax.nn.silu(c) @ w_ada[l] + b_ada[l]
        shift1, scale1, gate1, shift2, scale2, gate2 = jnp.split(ada[:, None, :], 6, axis=-1)
        u = (_layer_norm(x) * (1.0 + scale1) + shift1).astype(dt)
        proj = u @ w_in[l]
        gq, gk, gv, go, gr, dq, dk, dv, mg = jnp.split(proj, _split_points(), axis=-1)
        o_gla = _gla_branch(gq, gk, gv, go, gr, w_gla_gate2[l], b_gla_gate2[l], gla_norm_w[l])
        o_diff = _diff_branch(dq, dk, dv, positions, diff_lambda_q1[l], diff_lambda_k1[l],
                              diff_lambda_q2[l], diff_lambda_k2[l], diff_norm_w[l], lam_init)
        g_a, g_b = jnp.split(jax.nn.sigmoid(mg), 2, axis=-1)
        y = (g_a * o_gla + g_b * o_diff) @ w_out[l]
        x = (_layer_norm(alpha * x + gate1 * y) * ln1_w[l] + ln1_b[l]).astype(dt)
        u2 = (_layer_norm(x) * (1.0 + scale2) + shift2).astype(dt)
        m = _hier_moe(u2, w_router_group[l], b_router_group[l], w_router_expert[l], b_router_expert[l],
                      w_exp_gate[l], w_exp_up[l], w_exp_down[l])
        x = (_layer_norm(alpha * x + gate2 * m) * ln2_w[l] + ln2_b[l]).astype(dt)
    return x
```

```python
import math
import numpy as np
import concourse.bass as bass
import concourse.mybir as mybir
from concourse.bass_utils import run_bass_kernel_spmd

F32 = mybir.dt.float32
F32R = mybir.dt.float32r
BF16 = mybir.dt.bfloat16
I32 = mybir.dt.int32
AF = mybir.ActivationFunctionType
ALU = mybir.AluOpType
AX = mybir.AxisListType

D = 1024
NCH = 8
LN_EPS = 1e-5
RMS_EPS = 1e-6
ALPHA = 2.0 ** 0.25
LAM_INIT = 0.2
TWO_PI = 2.0 * math.pi
CW1 = 6.28125
CW2 = TWO_PI - CW1
PI_LO = 3.1415925
MAGIC = 12582912.0
BIG = 1.0e4
PIPE_MOE = True


class Prog:
    def __init__(self, nc, n_dma_sems=32):
        self.nc = nc
        self.eng = {"pe": nc.tensor, "act": nc.scalar, "dve": nc.vector,
                    "pool": nc.gpsimd, "sp": nc.sync}
        self.sem, self.cnt, self.ctx = {}, {}, []
        for name in self.eng:
            cm = nc.semaphore("s_" + name)
            self.sem[name] = cm.__enter__()
            self.ctx.append(cm)
            self.cnt[name] = 0
        self.dma_sems = []
        for i in range(n_dma_sems):
            cm = nc.semaphore("s_dma%d" % i)
            self.dma_sems.append(cm.__enter__())
            self.ctx.append(cm)
        self.dma_cnt = [0] * n_dma_sems
        self.dma_rr = 0
        self.waited = {name: {} for name in self.eng}
        self.snap = {name: [None] for name in self.eng}
        self.snap_cur = {name: {} for name in self.eng}
        self.dma_snap = {}
        self.state = {}
        self.n_instr = 0

    def close(self):
        for cm in reversed(self.ctx):
            cm.__exit__(None, None, None)

    def _semobj(self, k):
        return self.sem[k] if isinstance(k, str) else self.dma_sems[k]

    def _wait(self, engname, token):
        k, val = token
        w = self.waited[engname]
        if w.get(k, 0) >= val:
            return
        self.eng[engname].wait_ge(self._semobj(k), val)
        w[k] = val
        self.n_instr += 1
        if isinstance(k, str):
            other = self.snap[k][val] if val < len(self.snap[k]) else None
        else:
            other = self.dma_snap.get((k, val))
        if other:
            changed = False
            for ok, ov in other.items():
                if ok == engname:
                    continue
                if w.get(ok, 0) < ov:
                    w[ok] = ov
                    changed = True
        self.snap_cur[engname] = None

    def _deps(self, engname, reads, writes):
        toks = {}

        def add(tok):
            if tok is None:
                return
            k, v = tok
            if toks.get(k, 0) < v:
                toks[k] = v
        for k in reads:
            st = self.state.get(k)
            if st is not None:
                add(st[0])
        for k in writes:
            st = self.state.get(k)
            if st is not None:
                add(st[0])
                for rk, rv in st[1].items():
                    add((rk, rv))
        for k, v in toks.items():
            if engname == "pe" and k == "pe":
                continue
            self._wait(engname, (k, v))

    def _commit(self, token, reads, writes):
        for k in writes:
            self.state[k] = [token, {}]
        sk, sv = token
        for k in reads:
            st = self.state.get(k)
            if st is None:
                st = self.state[k] = [None, {}]
            if st[1].get(sk, 0) < sv:
                st[1][sk] = sv

    def op(self, engname, fn, reads=(), writes=()):
        self._deps(engname, reads, writes)
        ins = fn(self.eng[engname])
        self.cnt[engname] += 1
        if self.snap_cur[engname] is None:
            self.snap_cur[engname] = dict(self.waited[engname])
        self.snap[engname].append(self.snap_cur[engname])
        ins.then_inc(self.sem[engname], 1)
        self._commit((engname, self.cnt[engname]), reads, writes)
        self.n_instr += 1
        return ins

    def dma(self, out, in_, reads=(), writes=(), q="sp", **kw):
        self._deps(q, reads, writes)
        j = self.dma_rr
        self.dma_rr = (self.dma_rr + 1) % len(self.dma_sems)
        if self.dma_cnt[j] > 0:
            self._wait(q, (j, 16 * self.dma_cnt[j]))
        ins = self.eng[q].dma_start(out=out, in_=in_, **kw)
        self.dma_cnt[j] += 1
        ins.then_inc(self.dma_sems[j], 16)
        self.dma_snap[(j, 16 * self.dma_cnt[j])] = dict(self.waited[q])
        self._commit((j, 16 * self.dma_cnt[j]), reads, writes)
        self.n_instr += 1

    def barrier(self):
        for e in self.eng:
            for x in self.eng:
                if x != e and self.cnt[x] > 0:
                    self._wait(e, (x, self.cnt[x]))
            for j in range(len(self.dma_sems)):
                if self.dma_cnt[j] > 0:
                    self._wait(e, (j, 16 * self.dma_cnt[j]))
        self.state = {}

    def wait_all(self, engname, keys):
        for k in keys:
            st = self.state.get(k)
            if st is not None and st[0] is not None:
                self._wait(engname, st[0])


class Ring:
    def __init__(self, tiles, name):
        self.tiles = tiles
        self.name = name
        self.i = 0

    def next(self):
        j = self.i % len(self.tiles)
        self.i += 1
        return self.tiles[j], "%s%d" % (self.name, j)


def build_program(S, stage=99, dbg=False):
    NB = S // 512
    NT = S // 128
    TQ = min(1024, S)
    NQ = S // TQ
    nc = bass.Bass("TRN2", target_bir_lowering=False)

    def din(name, shape, dt=F32):
        return nc.dram_tensor(name, list(shape), dt, kind="ExternalInput").ap()

    xT = din("xT", [D, S])
    ccol = din("ccol", [128, 8])
    posb = din("posb", [128, S], I32)
    invf = din("invf", [128, 1])
    sgn = din("sgn", [128, 1])
    wada = din("wada", [128, 8, 6144])
    bada = din("bada", [1, 6144])
    wdiff = din("wdiff", [8, 128, 8, 512])
    wgla = din("wgla", [4, 128, 8, 1024])
    wgr = din("wgr", [128, 8, 16])
    wg2 = din("wg2", [16, 512])
    bg2c = din("bg2c", [128, 4])
    glanw = din("glanw", [128, 256])
    dnw = din("dnw", [128, 1])
    lamv = din("lamv", [128, 4, 64])
    wout = din("wout", [128, 8, 1024])
    lnp = din("lnp", [128, 4, 8])
    wr = din("wr", [128, 8, 36])
    br = din("br", [1, 36])
    if stage >= 5:
        wgu = din("wgu", [32, 128, 8, 1024])
        wd = din("wd", [32, 128, 4, 1024])
    outT = nc.dram_tensor("outT", [D, S], F32, kind="ExternalOutput").ap()
    mixd = nc.dram_tensor("mixd", [D, S], F32, kind="Internal").ap()
    mixg = nc.dram_tensor("mixg", [D, S], F32, kind="Internal").ap()
    x1T = nc.dram_tensor("x1T", [D, S], F32, kind="Internal").ap()
    u2T = nc.dram_tensor("u2T", [D, S], F32, kind="Internal").ap()

    def fm(ap):
        return ap.rearrange("(c p) t -> p c t", p=128)

    P = Prog(nc)
    from contextlib import ExitStack
    root = ExitStack()

    used_names = {}

    def sb(stack, name, shape, dt=F32):
        n = used_names.get(name, 0)
        used_names[name] = n + 1
        if n:
            name = "%s_r%d" % (name, n)
        return stack.enter_context(nc.sbuf_tensor(name, list(shape), dt))

    pb = [root.enter_context(nc.psum_tensor("pb%d" % i, [128, 512], F32)) for i in range(8)]
    pbk = ["pb%d" % i for i in range(8)]

    ident = sb(root, "ident", [128, 128])
    ones = sb(root, "ones", [128, 128])
    ones_r = sb(root, "ones_r", [128, 128], F32R)
    ident_r = sb(root, "ident_r", [128, 128], F32R)
    adaT = sb(root, "adaT", [128, 48])
    s1p = sb(root, "s1p", [128, 8])
    s2p = sb(root, "s2p", [128, 8])
    lnp_sb = sb(root, "lnp_sb", [128, 4, 8])
    Wts = sb(root, "Wts", [128, NT, 32])
    junk = sb(root, "junk", [128, 512])

    P.op("pool", lambda e: e.memset(ones[:], 1.0), writes=["ones"])
    P.op("pool", lambda e: e.memset(ident[:], 1.0), writes=["ident"])
    P.op("pool", lambda e: e.affine_select(out=ident[:], in_=ident[:], pattern=[[-1, 128]],
                                           compare_op=ALU.is_equal, fill=0.0, base=0,
                                           channel_multiplier=1), reads=["ident"], writes=["ident"])
    P.op("dve", lambda e: e.tensor_copy(out=ones_r[:], in_=ones[:]), reads=["ones"], writes=["ones_r"])
    P.op("dve", lambda e: e.tensor_copy(out=ident_r[:], in_=ident[:]), reads=["ident"], writes=["ident_r"])
    P.dma(lnp_sb[:], lnp, writes=["lnp_sb"])

    def ln_block(src, src_key, mean, rstd, sq, tag):
        P.op("act", lambda e: e.activation(out=sq[:], in_=src, func=AF.Square), reads=[src_key], writes=["sq"])
        for c in range(8):
            P.op("pe", lambda e: e.matmul(pb[0][:], lhsT=ones[:], rhs=src[:, c, :], start=(c == 0), stop=(c == 7)),
                 reads=["ones", src_key], writes=[pbk[0]])
        for c in range(8):
            P.op("pe", lambda e: e.matmul(pb[1][:], lhsT=ones_r[:], rhs=sq[:, c, :], start=(c == 0), stop=(c == 7)),
                 reads=["ones_r", "sq"], writes=[pbk[1]])
        P.op("dve", lambda e: e.tensor_scalar(out=mean[:], in0=pb[0][:], scalar1=1.0 / D, scalar2=None, op0=ALU.mult),
             reads=[pbk[0]], writes=["mean" + tag])
        P.op("dve", lambda e: e.tensor_tensor(out=rstd[:], in0=mean[:], in1=mean[:], op=ALU.mult),
             reads=["mean" + tag], writes=["rstd" + tag])
        P.op("dve", lambda e: e.scalar_tensor_tensor(out=rstd[:], in0=pb[1][:], scalar=1.0 / D, in1=rstd[:],
                                                      op0=ALU.mult, op1=ALU.subtract),
             reads=[pbk[1], "rstd" + tag], writes=["rstd" + tag])
        P.op("dve", lambda e: e.tensor_scalar(out=rstd[:], in0=rstd[:], scalar1=LN_EPS, scalar2=None, op0=ALU.add),
             reads=["rstd" + tag], writes=["rstd" + tag])
        P.op("act", lambda e: e.activation(out=rstd[:], in_=rstd[:], func=AF.Sqrt),
             reads=["rstd" + tag], writes=["rstd" + tag])
        P.op("dve", lambda e: e.reciprocal(out=rstd[:], in_=rstd[:]), reads=["rstd" + tag], writes=["rstd" + tag])

    def ln_apply(dst, dst_key, src, src_key, mean, rstd, tag):
        mb = mean[:].unsqueeze(1).to_broadcast([128, 8, 512])
        rb = rstd[:].unsqueeze(1).to_broadcast([128, 8, 512])
        P.op("dve", lambda e: e.tensor_tensor(out=dst, in0=src, in1=mb, op=ALU.subtract),
             reads=[src_key, "mean" + tag], writes=[dst_key])
        P.op("dve", lambda e: e.tensor_tensor(out=dst, in0=dst, in1=rb, op=ALU.mult),
             reads=[dst_key, "rstd" + tag], writes=[dst_key])

    _sc = nc.named_scope('st0'); _sc.__enter__()
    with ExitStack() as st0:
        cc = sb(st0, "cc", [128, 8])
        scb = sb(st0, "scb", [128, 8, 128])
        bada_sb = sb(st0, "bada_sb", [1, 6144])
        ada_bc = sb(st0, "ada_bc", [128, 6144])
        wring = Ring([sb(st0, "wada%d" % i, [128, 8, 512]) for i in range(2)], "wada")
        P.dma(cc[:], ccol, writes=["cc"])
        P.dma(bada_sb[:], bada, writes=["bada_sb"])
        P.op("act", lambda e: e.activation(out=cc[:], in_=cc[:], func=AF.Silu), reads=["cc"], writes=["cc"])
        for kc in range(8):
            P.op("dve", lambda e: e.tensor_copy(out=scb[:, kc, :], in_=cc[:, kc:kc + 1].to_broadcast([128, 128])),
                 reads=["cc"], writes=["scb"])
        for cg in range(12):
            wt, wk = wring.next()
            P.dma(wt[:], wada[:, :, cg * 512:(cg + 1) * 512], writes=[wk])
            bank = cg % 2
            for kc in range(8):
                P.op("pe", lambda e: e.matmul(pb[bank][:], lhsT=scb[:, kc, :], rhs=wt[:, kc, :], start=(kc == 0), stop=False),
                     reads=["scb", wk], writes=[pbk[bank]])
            P.op("pe", lambda e: e.matmul(pb[bank][:], lhsT=ones[0:1, :], rhs=bada_sb[0:1, cg * 512:(cg + 1) * 512],
                                          start=False, stop=True), reads=["ones", "bada_sb"], writes=[pbk[bank]])
            P.op("act", lambda e: e.copy(out=ada_bc[:, cg * 512:(cg + 1) * 512], in_=pb[bank][:]),
                 reads=[pbk[bank]], writes=["ada_bc"])
        for j in range(48):
            P.op("dve", lambda e: e.scalar_tensor_tensor(out=junk[:, 0:128], in0=ada_bc[:, j * 128:(j + 1) * 128], scalar=1.0,
                                                          in1=ident[:], op0=ALU.mult, op1=ALU.mult,
                                                          accum_out=adaT[:, j:j + 1]),
                 reads=["ada_bc", "ident"], writes=["junk", "adaT"])
        P.op("dve", lambda e: e.tensor_scalar(out=s1p[:], in0=adaT[:, 8:16], scalar1=1.0, scalar2=None, op0=ALU.add),
             reads=["adaT"], writes=["s1p"])
        P.op("dve", lambda e: e.tensor_scalar(out=s2p[:], in0=adaT[:, 32:40], scalar1=1.0, scalar2=None, op0=ALU.add),
             reads=["adaT"], writes=["s2p"])
    _sc.__exit__(None, None, None)
    SH1, G1, SH2, G2 = 0, 16, 24, 40
    P.barrier()

    mix_stack = ExitStack()
    uT = sb(mix_stack, "uT", [128, 8, S], BF16)

    _sc = nc.named_scope('st1'); _sc.__enter__()
    with ExitStack() as st1:
        xring = Ring([sb(st1, "xt%d" % i, [128, 8, 512]) for i in range(2)], "xt")
        sq = sb(st1, "sq", [128, 8, 512], F32R)
        mean = sb(st1, "mean", [128, 512])
        rstd = sb(st1, "rstd", [128, 512])
        for tb in range(NB):
            xt, xk = xring.next()
            P.dma(xt[:], fm(xT)[:, :, tb * 512:(tb + 1) * 512], writes=[xk])
            ln_block(xt[:], xk, mean, rstd, sq, "1")
            ln_apply(xt[:], xk, xt[:], xk, mean, rstd, "1")
            for c in range(8):
                P.op("act", lambda e: e.activation(out=uT[:, c, tb * 512:(tb + 1) * 512], in_=xt[:, c, :], func=AF.Identity,
                                                   scale=s1p[:, c:c + 1], bias=adaT[:, SH1 + c:SH1 + c + 1]),
                     reads=[xk, "s1p", "adaT"], writes=["uT%d" % tb])

    _sc.__exit__(None, None, None)
    P.barrier()
    if dbg and stage == 1:
        with ExitStack() as sd:
            tmp = sb(sd, "dbg_tmp", [128, 8, 512])
            for tb in range(NB):
                P.op("dve", lambda e: e.tensor_copy(out=tmp[:], in_=uT[:, :, tb * 512:(tb + 1) * 512]),
                     reads=["uT%d" % tb], writes=["dbg_tmp"])
                P.dma(fm(outT)[:, :, tb * 512:(tb + 1) * 512], tmp[:], reads=["dbg_tmp"], writes=["outT"])

    _sc = nc.named_scope('st2'); _sc.__enter__()
    if stage >= 2:
        with ExitStack() as st2:
            cosT = sb(st2, "cosT", [128, S])
            sinS = sb(st2, "sinS", [128, S])
            invf_sb = sb(st2, "invf_sb", [128, 1])
            sgn_sb = sb(st2, "sgn_sb", [128, 1])
            dnws = sb(st2, "dnws", [128, 1])
            neglam = sb(st2, "neglam", [128, 1])
            perm_r = sb(st2, "perm_r", [128, 128], F32R)
            maskA = sb(st2, "maskA", [128, 512])
            maskB = sb(st2, "maskB", [128, 512])
            P.dma(invf_sb[:], invf, writes=["invf_sb"])
            P.dma(sgn_sb[:], sgn, writes=["sgn_sb"])
            P.dma(dnws[:], dnw, writes=["dnws"])
            P.op("dve", lambda e: e.tensor_scalar(out=dnws[:], in0=dnws[:], scalar1=1.0 - LAM_INIT, scalar2=None, op0=ALU.mult),
                 reads=["dnws"], writes=["dnws"])
            for (d0, s0) in ((0, 32), (32, 0), (64, 96), (96, 64)):
                P.op("dve", lambda e: e.tensor_copy(out=perm_r[:, d0:d0 + 32], in_=ident[:, s0:s0 + 32]),
                     reads=["ident"], writes=["perm_r"])
            for (mt, mk, base) in ((maskA, "maskA", 0), (maskB, "maskB", -128)):
                P.op("pool", lambda e: e.memset(mt[:], 1.0), writes=[mk])
                for c in range(2):
                    P.op("pool", lambda e: e.affine_select(out=mt[:, c * 256:(c + 1) * 256], in_=mt[:, c * 256:(c + 1) * 256],
                                                           pattern=[[1, 256]], compare_op=ALU.is_ge, fill=0.0,
                                                           base=base, channel_multiplier=-1), reads=[mk], writes=[mk])
            with ExitStack() as sl:
                lv = sb(sl, "lv", [128, 4, 64])
                lacc = sb(sl, "lacc", [128, 2])
                P.dma(lv[:], lamv, writes=["lv"])
                for i in range(2):
                    P.op("dve", lambda e: e.scalar_tensor_tensor(out=junk[:, 0:64], in0=lv[:, 2 * i, :], scalar=1.0, in1=lv[:, 2 * i + 1, :],
                                                                  op0=ALU.mult, op1=ALU.mult, accum_out=lacc[:, i:i + 1]),
                         reads=["lv"], writes=["junk", "lacc"])
                P.op("act", lambda e: e.activation(out=lacc[:], in_=lacc[:], func=AF.Exp), reads=["lacc"], writes=["lacc"])
                P.op("dve", lambda e: e.tensor_tensor(out=neglam[:], in0=lacc[:, 1:2], in1=lacc[:, 0:1], op=ALU.subtract),
                     reads=["lacc"], writes=["neglam"])
                P.op("dve", lambda e: e.tensor_scalar(out=neglam[:], in0=neglam[:], scalar1=-LAM_INIT, scalar2=None, op0=ALU.add),
                     reads=["neglam"], writes=["neglam"])
            P.barrier()
            with ExitStack() as sr:
                posi = sb(sr, "posi", [128, S], I32)
                ang = sb(sr, "ang", [128, S])
                kf = sb(sr, "kf", [128, S])
                P.dma(posi[:], posb, writes=["posi"])
                P.op("dve", lambda e: e.tensor_copy(out=ang[:], in_=posi[:]), reads=["posi"], writes=["ang"])
                P.op("dve", lambda e: e.tensor_scalar(out=ang[:], in0=ang[:], scalar1=invf_sb[:, 0:1], scalar2=None, op0=ALU.mult),
                     reads=["ang", "invf_sb"], writes=["ang"])
                for (dst, dk_, off, post) in ((sinS, "sinS", 0.0, 0.0), (cosT, "cosT", 0.25, math.pi / 2)):
                    P.op("dve", lambda e: e.tensor_scalar(out=kf[:], in0=ang[:], scalar1=1.0 / TWO_PI, scalar2=off,
                                                          op0=ALU.mult, op1=ALU.add), reads=["ang"], writes=["kf"])
                    P.op("dve", lambda e: e.tensor_scalar(out=kf[:], in0=kf[:], scalar1=MAGIC, scalar2=None, op0=ALU.add),
                         reads=["kf"], writes=["kf"])
                    P.op("dve", lambda e: e.tensor_scalar(out=kf[:], in0=kf[:], scalar1=-MAGIC, scalar2=None, op0=ALU.add),
                         reads=["kf"], writes=["kf"])
                    P.op("dve", lambda e: e.scalar_tensor_tensor(out=dst[:], in0=kf[:], scalar=-CW1, in1=ang[:],
                                                                  op0=ALU.mult, op1=ALU.add), reads=["kf", "ang"], writes=[dk_])
                    P.op("dve", lambda e: e.scalar_tensor_tensor(out=dst[:], in0=kf[:], scalar=-CW2, in1=dst[:],
                                                                  op0=ALU.mult, op1=ALU.add), reads=["kf", dk_], writes=[dk_])
                    P.op("dve", lambda e: e.tensor_scalar(out=dst[:], in0=dst[:], scalar1=post, scalar2=PI_LO,
                                                          op0=ALU.add, op1=ALU.min), reads=[dk_], writes=[dk_])
                    P.op("dve", lambda e: e.tensor_scalar(out=dst[:], in0=dst[:], scalar1=-PI_LO, scalar2=None, op0=ALU.max),
                         reads=[dk_], writes=[dk_])
                    P.op("act", lambda e: e.activation(out=dst[:], in_=dst[:], func=AF.Sin), reads=[dk_], writes=[dk_])
                P.op("dve", lambda e: e.tensor_scalar(out=sinS[:], in0=sinS[:], scalar1=sgn_sb[:, 0:1], scalar2=None, op0=ALU.mult),
                     reads=["sinS", "sgn_sb"], writes=["sinS"])

            P.barrier()
            wbfs = [sb(st2, "wbf%d" % i, [128, 8, 512], BF16) for i in range(2)]
            kT = sb(st2, "kT", [128, S], F32R)
            vtok = sb(st2, "vtok", [128, NT, 128], F32R)
            qexps = [sb(st2, "qexp%d" % i, [128, 2, 512], F32R) for i in range(2)]
            gbTs = [sb(st2, "gbT%d" % i, [128, 512]) for i in range(2)]
            rawr = Ring([sb(st2, "raw%d" % i, [128, 512], F32R) for i in range(2)], "raw")
            t1r = Ring([sb(st2, "t1_%d" % i, [128, 512]) for i in range(2)], "t1_")
            t2r = Ring([sb(st2, "t2_%d" % i, [128, 512]) for i in range(2)], "t2_")
            vTs = sb(st2, "vTs", [128, 512])
            pring = Ring([sb(st2, "pT%d" % i, [128, 512], F32R) for i in range(5)], "pT")
            rz = sb(st2, "rz", [128, 512])
            on = sb(st2, "on", [128, 512])
            od = sb(st2, "od", [128, 256])
            osq = sb(st2, "osq", [128, 256], F32R)
            rn = sb(st2, "rn", [128, 256])
            mo = sb(st2, "mo", [128, 256])
            for i in range(2):
                P.op("pool", lambda e: e.memset(qexps[i][:].bitcast(F32), 0.0), writes=["qexp%d" % i])
            PJ = (0, 1)
            PP, PO, PZ = 2, 5, 6
            PN = PP
            sring = Ring([pb[3], pb[4], pb[7]], "psS")
            LOOK = 2
            PVDELAY = 1

            def load_w(h):
                P.dma(wbfs[h % 2][:], wdiff[h], writes=["wbf%d" % (h % 2)], q="pool")

            def proj_gen(n, h, tb):
                p = n % 2
                wbf, wk = wbfs[h % 2], "wbf%d" % (h % 2)
                qexp, qk = qexps[p], "qexp%d" % p
                gbT, gk = gbTs[p], "gbT%d" % p
                ts_ = slice(tb * 512, (tb + 1) * 512)
                uk = "uT%d" % tb
                for which in (1, 0):
                    bank = PJ[which]
                    for kc in range(8):
                        P.op("pe", lambda e: e.matmul(pb[bank][:], lhsT=wbf[:, kc, which * 128:(which + 1) * 128],
                                                      rhs=uT[:, kc, ts_], start=(kc == 0), stop=(kc == 7)),
                             reads=[wk, uk], writes=[pbk[bank]])
                    yield
                    raw, rk = rawr.next()
                    t1, t1k = t1r.next()
                    t2, t2k = t2r.next()
                    P.op("act", lambda e: e.activation(out=raw[:], in_=pb[bank][:], func=AF.Copy,
                                                       scale=(0.125 if which == 0 else 1.0)),
                         reads=[pbk[bank]], writes=[rk])
                    P.op("pe", lambda e: e.matmul(pb[PP][:], lhsT=perm_r[:], rhs=raw[:], start=True, stop=True),
                         reads=["perm_r", rk], writes=[pbk[PP]])
                    P.op("pool", lambda e: e.tensor_tensor(out=t1[:], in0=raw[:].bitcast(F32), in1=cosT[:, ts_], op=ALU.mult),
                         reads=[rk, "cosT"], writes=[t1k])
                    P.op("dve", lambda e: e.tensor_tensor(out=t2[:], in0=pb[PP][:], in1=sinS[:, ts_], op=ALU.mult),
                         reads=[pbk[PP], "sinS"], writes=[t2k])
                    if which == 0:
                        for c in range(2):
                            ps_ = slice(c * 64, (c + 1) * 64)
                            P.op("dve", lambda e: e.tensor_tensor(
                                out=qexp[ps_, :, c * 256:(c + 1) * 256],
                                in0=t1[ps_, :].rearrange("p (j q) -> p j q", q=256),
                                in1=t2[ps_, :].rearrange("p (j q) -> p j q", q=256), op=ALU.add),
                                reads=[t1k, t2k], writes=[qk])
                    else:
                        P.op("dve", lambda e: e.tensor_tensor(out=kT[:, ts_], in0=t1[:], in1=t2[:], op=ALU.add),
                             reads=[t1k, t2k], writes=["kT%d" % tb])
                    yield
                for kc in range(8):
                    P.op("pe", lambda e: e.matmul(pb[PJ[0]][:], lhsT=wbf[:, kc, 256:384], rhs=uT[:, kc, ts_],
                                                  start=(kc == 0), stop=(kc == 7)), reads=[wk, uk], writes=[pbk[PJ[0]]])
                yield
                P.op("act", lambda e: e.copy(out=vTs[:], in_=pb[PJ[0]][:]), reads=[pbk[PJ[0]]], writes=["vTs"])
                for kc in range(8):
                    P.op("pe", lambda e: e.matmul(pb[PJ[1]][:], lhsT=wbf[:, kc, 384:512], rhs=uT[:, kc, ts_],
                                                  start=(kc == 0), stop=(kc == 7)), reads=[wk, uk], writes=[pbk[PJ[1]]])
                P.op("act", lambda e: e.activation(out=gbT[:], in_=pb[PJ[1]][:], func=AF.Sigmoid),
                     reads=[pbk[PJ[1]]], writes=[gk])
                yield
                for j in range(4):
                    P.op("pe", lambda e: e.transpose(pb[PP][:, j * 128:(j + 1) * 128], vTs[:, j * 128:(j + 1) * 128], ident[:]),
                         reads=["vTs", "ident"], writes=[pbk[PP]])
                P.op("act", lambda e: e.copy(out=vtok[:, tb * 4:(tb + 1) * 4, :],
                                             in_=pb[PP][:].rearrange("p (j d) -> p j d", d=128)),
                     reads=[pbk[PP]], writes=["vtok%d" % tb])
                yield

            def attn_gen(n, h, tb):
                p = n % 2
                qexp, qk = qexps[p], "qexp%d" % p
                gbT, gk = gbTs[p], "gbT%d" % p
                for jl in range(2):
                    jb = tb * 2 + jl
                    nk = 2 * jb + 2
                    sbanks = {}

                    def qk_mm(i):
                        sps, spk = sring.next()
                        sbanks[i] = (sps, spk)
                        P.op("pe", lambda e: e.matmul(sps[:], lhsT=kT[:, i * 128:(i + 1) * 128], rhs=qexp[:, jl, :],
                                                      start=True, stop=True),
                             reads=["kT%d" % (i // 4), qk], writes=[spk])
                    pend_pv = []

                    def pv_mm(i, pt, ptk):
                        P.op("pe", lambda e: e.matmul(pb[PO][:], lhsT=vtok[:, i, :], rhs=pt[:], start=(i == 0), stop=(i == nk - 1)),
                             reads=["vtok%d" % (i // 4), ptk], writes=[pbk[PO]])
                        P.op("pe", lambda e: e.matmul(pb[PZ][:], lhsT=ones_r[:], rhs=pt[:], start=(i == 0), stop=(i == nk - 1)),
                             reads=["ones_r", ptk], writes=[pbk[PZ]])
                    for i in range(min(LOOK, nk)):
                        qk_mm(i)
                    for i in range(nk):
                        if i + LOOK < nk:
                            qk_mm(i + LOOK)
                        sps, spk = sbanks.pop(i)
                        pt, ptk = pring.next()
                        P.op("act", lambda e: e.activation(out=pt[:], in_=sps[:], func=AF.Exp), reads=[spk], writes=[ptk])
                        if i >= 2 * jb:
                            mt, mk = (maskA, "maskA") if i == 2 * jb else (maskB, "maskB")
                            P.op("dve", lambda e: e.tensor_tensor(out=pt[:], in0=pt[:].bitcast(F32), in1=mt[:], op=ALU.mult),
                                 reads=[ptk, mk], writes=[ptk])
                        pend_pv.append((i, pt, ptk))
                        if len(pend_pv) > PVDELAY:
                            pv_mm(*pend_pv.pop(0))
                        yield
                    while pend_pv:
                        pv_mm(*pend_pv.pop(0))
                    P.op("dve", lambda e: e.reciprocal(out=rz[:], in_=pb[PZ][:]), reads=[pbk[PZ]], writes=["rz"])
                    P.op("dve", lambda e: e.tensor_tensor(out=on[:], in0=pb[PO][:], in1=rz[:], op=ALU.mult),
                         reads=[pbk[PO], "rz"], writes=["on"])
                    P.op("dve", lambda e: e.scalar_tensor_tensor(out=od[:], in0=on[:, 256:512], scalar=neglam[:, 0:1],
                                                                  in1=on[:, 0:256], op0=ALU.mult, op1=ALU.add),
                         reads=["on", "neglam"], writes=["od"])
                    P.op("act", lambda e: e.activation(out=osq[:], in_=od[:], func=AF.Square), reads=["od"], writes=["osq"])
                    P.op("pe", lambda e: e.matmul(pb[PN][:, 0:256], lhsT=ones_r[:], rhs=osq[:], start=True, stop=True),
                         reads=["ones_r", "osq"], writes=[pbk[PN]])
                    P.op("dve", lambda e: e.tensor_scalar(out=rn[:], in0=pb[PN][:, 0:256], scalar1=1.0 / 128, scalar2=RMS_EPS,
                                                          op0=ALU.mult, op1=ALU.add), reads=[pbk[PN]], writes=["rn"])
                    P.op("act", lambda e: e.activation(out=rn[:], in_=rn[:], func=AF.Sqrt), reads=["rn"], writes=["rn"])
                    P.op("dve", lambda e: e.reciprocal(out=rn[:], in_=rn[:]), reads=["rn"], writes=["rn"])
                    P.op("dve", lambda e: e.scalar_tensor_tensor(out=mo[:], in0=od[:], scalar=dnws[:, 0:1], in1=rn[:],
                                                                  op0=ALU.mult, op1=ALU.mult),
                         reads=["od", "dnws", "rn"], writes=["mo"])
                    P.op("dve", lambda e: e.tensor_tensor(out=mo[:], in0=mo[:], in1=gbT[:, jl * 256:(jl + 1) * 256], op=ALU.mult),
                         reads=["mo", gk], writes=["mo"])
                    P.dma(mixd[h * 128:(h + 1) * 128, jb * 256:(jb + 1) * 256], mo[:], reads=["mo"], writes=["mixd"])
                    yield

            def run_all(g):
                for _ in g:
                    pass

            def interleave(ga, gb_):
                gens = [ga, gb_]
                while gens:
                    for g in list(gens):
                        try:
                            next(g)
                        except StopIteration:
                            gens.remove(g)

            items = [(h, tb) for h in range(8) for tb in range(NB)]
            load_w(0)
            if len(items) > 0:
                load_w(1)
            run_all(proj_gen(0, 0, 0))
            for n, (h, tb) in enumerate(items):
                nxt = items[n + 1] if n + 1 < len(items) else None
                if nxt is not None and nxt[0] == h:
                    interleave(attn_gen(n, h, tb), proj_gen(n + 1, nxt[0], nxt[1]))
                else:
                    run_all(attn_gen(n, h, tb))
                    if h + 2 < 8:
                        load_w(h + 2)
                    if nxt is not None:
                        run_all(proj_gen(n + 1, nxt[0], nxt[1]))

    _sc.__exit__(None, None, None)
    if dbg == 1 and stage == 2:
        with ExitStack() as sd:
            tmp = sb(sd, "dbg_tmp2", [128, 8, 512])
            for tb in range(NB):
                P.dma(tmp[:], fm(mixd)[:, :, tb * 512:(tb + 1) * 512], reads=["mixd"], writes=["dbg_tmp2"])
                P.dma(fm(outT)[:, :, tb * 512:(tb + 1) * 512], tmp[:], reads=["dbg_tmp2"], writes=["outT"])


    P.barrier()
    _sc = nc.named_scope('st3'); _sc.__enter__()
    if stage >= 3:
        with ExitStack() as st3:
            cmask = sb(st3, "cmask", [128, 512])
            mask2 = sb(st3, "mask2", [128, 128])
            wg2_sb = sb(st3, "wg2_sb", [16, 512])
            negb = sb(st3, "negb", [128, 4])
            glanw_sb = sb(st3, "glanw_sb", [128, 256])
            wgr_st = sb(st3, "wgr_st", [128, 8, 16])
            wgr_bf = sb(st3, "wgr_bf", [128, 8, 16], BF16)
            glrT = sb(st3, "glrT", [16, S])
            P.op("pool", lambda e: e.memset(cmask[:], 1.0), writes=["cmask"])
            P.op("pool", lambda e: e.memset(cmask[:].rearrange("p (n c) -> p n c", c=64)[:, :, 0:1], 0.0),
                 reads=["cmask"], writes=["cmask"])
            P.op("pool", lambda e: e.memset(mask2[:], 1.0), writes=["mask2"])
            P.op("pool", lambda e: e.affine_select(out=mask2[:], in_=mask2[:], pattern=[[1, 128]], compare_op=ALU.is_ge,
                                                   fill=0.0, base=0, channel_multiplier=-1), reads=["mask2"], writes=["mask2"])
            P.op("pool", lambda e: e.memset(mask2[0:64, 64:128], 0.0), reads=["mask2"], writes=["mask2"])
            P.dma(wg2_sb[:], wg2, writes=["wg2_sb"])
            P.dma(negb[:], bg2c, writes=["negb"])
            P.op("dve", lambda e: e.tensor_scalar(out=negb[:], in0=negb[:], scalar1=-1.0, scalar2=None, op0=ALU.mult),
                 reads=["negb"], writes=["negb"])
            P.dma(glanw_sb[:], glanw, writes=["glanw_sb"])
            P.dma(wgr_st[:], wgr, writes=["wgr_st"])
            P.op("dve", lambda e: e.tensor_copy(out=wgr_bf[:], in_=wgr_st[:]), reads=["wgr_st"], writes=["wgr_bf"])
            for tb in range(NB):
                ts_ = slice(tb * 512, (tb + 1) * 512)
                for kc in range(8):
                    P.op("pe", lambda e: e.matmul(pb[0][0:16, :], lhsT=wgr_bf[:, kc, :], rhs=uT[:, kc, ts_],
                                                  start=(kc == 0), stop=(kc == 7)), reads=["wgr_bf", "uT%d" % tb], writes=[pbk[0]])
                P.op("act", lambda e: e.copy(out=glrT[:, ts_], in_=pb[0][0:16, :]), reads=[pbk[0]], writes=["glrT%d" % tb])

            def mkslot(sl):
                B = {}
                B["wbf2"] = sb(st3, "wbf2_%d" % sl, [128, 8, 1024], BF16)
                B["st"] = [sb(st3, "stA_%d" % sl, [128, 256]), sb(st3, "stB_%d" % sl, [128, 256])]
                for nm in ("spl", "cs", "eb", "enb", "ed", "qt", "kt", "kd"):
                    B[nm] = sb(st3, "%s_%d" % (nm, sl), [128, 512])
                B["dec"] = sb(st3, "dec_%d" % sl, [128, 8])
                B["kdt"] = sb(st3, "kdt_%d" % sl, [128, 4, 128])
                for nm in ("vt", "sog", "sga"):
                    B[nm] = sb(st3, "%s_%d" % (nm, sl), [128, 4, 256])
                B["am"] = sb(st3, "am_%d" % sl, [128, 128])
                B["ssq"] = sb(st3, "ssq_%d" % sl, [128, 1])
                B["rn1"] = sb(st3, "rn1_%d" % sl, [128, 1])
                B["res_"] = sb(st3, "res__%d" % sl, [128, 256])
                B["mgT"] = sb(st3, "mgT_%d" % sl, [128, 2, 512])
                B["junk"] = sb(st3, "junk3_%d" % sl, [128, 256])
                return B
            slots = [mkslot(0), mkslot(1)]

            def gla_gen(h, sl):
                B = slots[sl]
                K_ = lambda nm: "%s_%d" % (nm, sl)
                bq_, bk_, bm_, bo_ = 4 * sl, 4 * sl + 1, 4 * sl + 2, 4 * sl + 3
                wbf2, stt_ = B["wbf2"], B["st"]
                spl, cs, eb, enb, ed, dec = B["spl"], B["cs"], B["eb"], B["enb"], B["ed"], B["dec"]
                qt, kt, kd, kdt, vt, sog, sga = B["qt"], B["kt"], B["kd"], B["kdt"], B["vt"], B["sog"], B["sga"]
                am, ssq, rn1, res_, mgT, jk = B["am"], B["ssq"], B["rn1"], B["res_"], B["mgT"], B["junk"]
                cs3 = cs[:].rearrange("p (n c) -> p n c", c=64)
                P.dma(wbf2[:], wgla[h], writes=[K_("wbf2")], q="pool")
                cur = 0
                P.op("dve", lambda e: e.memset(stt_[0][:], 0.0), writes=[K_("st0")])
                for tb in range(NB):
                    ts_ = slice(tb * 512, (tb + 1) * 512)
                    uk = "uT%d" % tb
                    for which in range(2):
                        for kc in range(8):
                            P.op("pe", lambda e: e.matmul(pb[4 * sl + which][:], lhsT=wbf2[:, kc, which * 128:(which + 1) * 128],
                                                          rhs=uT[:, kc, ts_], start=(kc == 0), stop=(kc == 7)),
                                 reads=[K_("wbf2"), uk], writes=[pbk[4 * sl + which]])
                    P.op("pe", lambda e: e.matmul(pb[bm_][:], lhsT=wg2_sb[0:16, h * 128:(h + 1) * 128], rhs=glrT[0:16, ts_],
                                                  start=True, stop=True), reads=["wg2_sb", "glrT%d" % tb], writes=[pbk[bm_]])
                    yield
                    P.op("act", lambda e: e.activation(out=spl[:], in_=pb[bm_][:], func=AF.Exp, scale=-1.0, bias=negb[:, h:h + 1]),
                         reads=[pbk[bm_], "negb"], writes=[K_("spl")])
                    P.op("act", lambda e: e.activation(out=spl[:], in_=spl[:], func=AF.Ln, bias=1.0), reads=[K_("spl")], writes=[K_("spl")])
                    P.op("dve", lambda e: e.tensor_tensor_scan(out=cs[:], data0=cmask[:], data1=spl[:], initial=0.0,
                                                               op0=ALU.mult, op1=ALU.add), reads=["cmask", K_("spl")], writes=[K_("cs")])
                    yield
                    P.op("act", lambda e: e.activation(out=eb[:], in_=cs[:], func=AF.Exp, scale=-1.0 / 16), reads=[K_("cs")], writes=[K_("eb")])
                    P.op("act", lambda e: e.activation(out=enb[:], in_=cs[:], func=AF.Exp, scale=1.0 / 16), reads=[K_("cs")], writes=[K_("enb")])
                    P.op("dve", lambda e: e.tensor_tensor(out=ed[:].rearrange("p (n c) -> p n c", c=64), in0=cs3,
                                                          in1=cs3[:, :, 63:64].to_broadcast([128, 8, 64]), op=ALU.subtract),
                         reads=[K_("cs")], writes=[K_("ed")])
                    P.op("act", lambda e: e.activation(out=ed[:], in_=ed[:], func=AF.Exp, scale=1.0 / 16), reads=[K_("ed")], writes=[K_("ed")])
                    P.op("act", lambda e: e.activation(out=dec[:], in_=cs3[:, :, 63], func=AF.Exp, scale=-1.0 / 16),
                         reads=[K_("cs")], writes=[K_("dec")])
                    yield
                    P.op("dve", lambda e: e.scalar_tensor_tensor(out=qt[:], in0=pb[bq_][:], scalar=128.0 ** -0.5, in1=eb[:],
                                                                  op0=ALU.mult, op1=ALU.mult), reads=[pbk[bq_], K_("eb")], writes=[K_("qt")])
                    P.op("dve", lambda e: e.tensor_tensor(out=kt[:], in0=pb[bk_][:], in1=enb[:], op=ALU.mult),
                         reads=[pbk[bk_], K_("enb")], writes=[K_("kt")])
                    P.op("dve", lambda e: e.tensor_tensor(out=kd[:], in0=pb[bk_][:], in1=ed[:], op=ALU.mult),
                         reads=[pbk[bk_], K_("ed")], writes=[K_("kd")])
                    yield
                    for j in range(4):
                        P.op("pe", lambda e: e.transpose(pb[bm_][:, j * 128:(j + 1) * 128], kd[:, j * 128:(j + 1) * 128], ident[:]),
                             reads=[K_("kd"), "ident"], writes=[pbk[bm_]])
                    P.op("act", lambda e: e.copy(out=kdt[:], in_=pb[bm_][:].rearrange("p (j d) -> p j d", d=128)),
                         reads=[pbk[bm_]], writes=[K_("kdt")])
                    yield
                    for j in range(4):
                        tj = slice(tb * 512 + j * 128, tb * 512 + (j + 1) * 128)
                        for kc in range(8):
                            P.op("pe", lambda e: e.matmul(pb[bq_][:], lhsT=uT[:, kc, tj], rhs=wbf2[:, kc, 256:768],
                                                          start=(kc == 0), stop=(kc == 7)), reads=[K_("wbf2"), uk], writes=[pbk[bq_]])
                        for kc in range(8):
                            P.op("pe", lambda e: e.matmul(pb[bk_][:, 0:256], lhsT=uT[:, kc, tj], rhs=wbf2[:, kc, 768:1024],
                                                          start=(kc == 0), stop=(kc == 7)), reads=[K_("wbf2"), uk], writes=[pbk[bk_]])
                        P.op("act", lambda e: e.copy(out=vt[:, j, :], in_=pb[bq_][:, 0:256]), reads=[pbk[bq_]], writes=[K_("vt%d" % j)])
                        P.op("act", lambda e: e.activation(out=sog[:, j, :], in_=pb[bq_][:, 256:512], func=AF.Silu),
                             reads=[pbk[bq_]], writes=[K_("sog%d" % j)])
                        P.op("act", lambda e: e.activation(out=sga[:, j, :], in_=pb[bk_][:, 0:256], func=AF.Sigmoid),
                             reads=[pbk[bk_]], writes=[K_("sga%d" % j)])
                        yield
                    for j in range(4):
                        js = slice(j * 128, (j + 1) * 128)
                        P.op("pe", lambda e: e.matmul(pb[bm_][:, 256:384], lhsT=kt[:, js], rhs=qt[:, js], start=True, stop=True),
                             reads=[K_("kt"), K_("qt")], writes=[pbk[bm_]])
                        P.op("dve", lambda e: e.tensor_tensor(out=am[:], in0=pb[bm_][:, 256:384], in1=mask2[:], op=ALU.mult),
                             reads=[pbk[bm_], "mask2"], writes=[K_("am")])
                        yield
                        P.op("pe", lambda e: e.matmul(pb[bo_][:, 0:256], lhsT=am[:], rhs=vt[:, j, :], start=True, stop=False),
                             reads=[K_("am"), K_("vt%d" % j)], writes=[pbk[bo_]])
                        for half in range(2):
                            n = 2 * j + half
                            hs = slice(half * 64, (half + 1) * 64)
                            P.op("pe", lambda e: e.matmul(pb[bo_][hs, 0:256], lhsT=qt[:, n * 64:(n + 1) * 64], rhs=stt_[cur][:],
                                                          start=False, stop=(half == 1)),
                                 reads=[K_("qt"), K_("st%d" % cur)], writes=[pbk[bo_]])
                            if half == 0:
                                P.op("pe", lambda e: e.matmul(pb[bm_][:, 0:256], lhsT=kdt[hs, j, :], rhs=vt[hs, j, :], start=True, stop=True),
                                     reads=[K_("kdt"), K_("vt%d" % j)], writes=[pbk[bm_]])
                                P.op("dve", lambda e: e.scalar_tensor_tensor(out=stt_[1 - cur][:], in0=stt_[cur][:], scalar=dec[:, n:n + 1],
                                                                              in1=pb[bm_][:, 0:256], op0=ALU.mult, op1=ALU.add),
                                     reads=[K_("st%d" % cur), K_("dec"), pbk[bm_]], writes=[K_("st%d" % (1 - cur))])
                                cur = 1 - cur
                                yield
                        P.op("act", lambda e: e.activation(out=jk[:], in_=pb[bo_][:, 0:256], func=AF.Square, accum_out=ssq[:]),
                             reads=[pbk[bo_]], writes=[K_("junk"), K_("ssq")])
                        P.op("dve", lambda e: e.tensor_scalar(out=rn1[:], in0=ssq[:], scalar1=1.0 / 256, scalar2=RMS_EPS,
                                                              op0=ALU.mult, op1=ALU.add), reads=[K_("ssq")], writes=[K_("rn1")])
                        P.op("act", lambda e: e.activation(out=rn1[:], in_=rn1[:], func=AF.Sqrt), reads=[K_("rn1")], writes=[K_("rn1")])
                        P.op("dve", lambda e: e.reciprocal(out=rn1[:], in_=rn1[:]), reads=[K_("rn1")], writes=[K_("rn1")])
                        P.op("dve", lambda e: e.scalar_tensor_tensor(out=res_[:], in0=pb[bo_][:, 0:256], scalar=rn1[:, 0:1], in1=glanw_sb[:],
                                                                      op0=ALU.mult, op1=ALU.mult),
                             reads=[pbk[bo_], K_("rn1"), "glanw_sb"], writes=[K_("res_")])
                        hs = slice(64, 128)
                        n = 2 * j + 1
                        P.op("pe", lambda e: e.matmul(pb[bm_][:, 0:256], lhsT=kdt[hs, j, :], rhs=vt[hs, j, :], start=True, stop=True),
                             reads=[K_("kdt"), K_("vt%d" % j)], writes=[pbk[bm_]])
                        P.op("dve", lambda e: e.scalar_tensor_tensor(out=stt_[1 - cur][:], in0=stt_[cur][:], scalar=dec[:, n:n + 1],
                                                                      in1=pb[bm_][:, 0:256], op0=ALU.mult, op1=ALU.add),
                             reads=[K_("st%d" % cur), K_("dec"), pbk[bm_]], writes=[K_("st%d" % (1 - cur))])
                        cur = 1 - cur
                        yield
                        P.op("dve", lambda e: e.tensor_tensor(out=res_[:], in0=res_[:], in1=sog[:, j, :], op=ALU.mult),
                             reads=[K_("res_"), K_("sog%d" % j)], writes=[K_("res_")])
                        P.op("dve", lambda e: e.tensor_tensor(out=res_[:], in0=res_[:], in1=sga[:, j, :], op=ALU.mult),
                             reads=[K_("res_"), K_("sga%d" % j)], writes=[K_("res_")])
                        for f in range(2):
                            P.op("pe", lambda e: e.transpose(pb[bk_][:, 256 + f * 128:256 + (f + 1) * 128],
                                                             res_[:, f * 128:(f + 1) * 128], ident[:]),
                                 reads=[K_("res_"), "ident"], writes=[pbk[bk_]])
                        P.op("act", lambda e: e.copy(out=mgT[:, :, js], in_=pb[bk_][:, 256:512].rearrange("p (f t) -> p f t", t=128)),
                             reads=[pbk[bk_]], writes=[K_("mgT")])
                        yield
                    P.dma(mixg[h * 256:(h + 1) * 256, ts_].rearrange("(f p) t -> p f t", p=128), mgT[:],
                          reads=[K_("mgT")], writes=["mixg"])
                    yield

            def interleave3(gens):
                gens = list(gens)
                while gens:
                    for g in list(gens):
                        try:
                            next(g)
                        except StopIteration:
                            gens.remove(g)
            for hp in range(2):
                interleave3([gla_gen(2 * hp, 0), gla_gen(2 * hp + 1, 1)])

    _sc.__exit__(None, None, None)
    if dbg == 1 and stage == 3:
        with ExitStack() as sd:
            tmp = sb(sd, "dbg_tmp3", [128, 8, 512])
            for tb in range(NB):
                P.dma(tmp[:], fm(mixg)[:, :, tb * 512:(tb + 1) * 512], reads=["mixg"], writes=["dbg_tmp3"])
                P.dma(fm(outT)[:, :, tb * 512:(tb + 1) * 512], tmp[:], reads=["dbg_tmp3"], writes=["outT"])


    P.barrier()
    mix_stack.close()
    _sc = nc.named_scope('st4'); _sc.__enter__()
    if stage >= 4:
        with ExitStack() as st4:
            wout_r = sb(st4, "wout_r", [128, 8, 1024], F32R)
            wr_sb = sb(st4, "wr_sb", [128, 8, 36])
            br_sb = sb(st4, "br_sb", [1, 36])
            mdb = sb(st4, "mdb", [128, 8, 512])
            mgb = sb(st4, "mgb", [128, 8, 512])
            mixr = sb(st4, "mixr", [128, 8, 512], F32R)
            xt4 = sb(st4, "xt4", [128, 8, 512])
            x1b = sb(st4, "x1b", [128, 8, 512])
            sq = sb(st4, "sq4", [128, 8, 512], F32R)
            mean = sb(st4, "mean4", [128, 512])
            rstd = sb(st4, "rstd4", [128, 512])
            L = sb(st4, "L", [128, 4, 36])
            r4 = [sb(st4, "r4_%d" % i, [128, 4]) for i in range(8)]
            g44 = sb(st4, "g44", [128, 4, 4])
            lem = sb(st4, "lem", [128, 4, 32])
            lem2 = sb(st4, "lem2", [128, 4, 32])
            m1 = sb(st4, "m1", [128, 4, 32])
            m2 = sb(st4, "m2", [128, 4, 32])
            P.dma(wout_r[:], wout, writes=["wout_r"], q="pool")
            P.dma(wr_sb[:], wr, writes=["wr_sb"])
            P.dma(br_sb[:], br, writes=["br_sb"])

            def bc4(t, n):
                return t[:].unsqueeze(2).to_broadcast([128, 4, n])

            for tb in range(NB):
                ts_ = slice(tb * 512, (tb + 1) * 512)
                P.dma(mdb[:], fm(mixd)[:, :, ts_], reads=["mixd"], writes=["mdb"])
                P.dma(mgb[:], fm(mixg)[:, :, ts_], reads=["mixg"], writes=["mgb"])
                P.dma(xt4[:], fm(xT)[:, :, ts_], writes=["xt4"])
                P.op("dve", lambda e: e.tensor_tensor(out=mixr[:], in0=mdb[:], in1=mgb[:], op=ALU.add),
                     reads=["mdb", "mgb"], writes=["mixr"])
                P.op("act", lambda e: e.mul(out=xt4[:], in_=xt4[:], mul=ALPHA), reads=["xt4"], writes=["xt4"])
                for dc in range(8):
                    bank = 2 + dc % 2
                    for kc in range(8):
                        P.op("pe", lambda e: e.matmul(pb[bank][:], lhsT=wout_r[:, kc, dc * 128:(dc + 1) * 128], rhs=mixr[:, kc, :],
                                                      start=(kc == 0), stop=(kc == 7)), reads=["wout_r", "mixr"], writes=[pbk[bank]])
                    P.op("dve", lambda e: e.scalar_tensor_tensor(out=xt4[:, dc, :], in0=pb[bank][:], scalar=adaT[:, G1 + dc:G1 + dc + 1],
                                                                  in1=xt4[:, dc, :], op0=ALU.mult, op1=ALU.add),
                         reads=[pbk[bank], "adaT", "xt4"], writes=["xt4"])
                ln_block(xt4[:], "xt4", mean, rstd, sq, "4")
                ln_apply(xt4[:], "xt4", xt4[:], "xt4", mean, rstd, "4")
                for dc in range(8):
                    P.op("act", lambda e: e.activation(out=x1b[:, dc, :], in_=xt4[:, dc, :], func=AF.Identity,
                                                       scale=lnp_sb[:, 0, dc:dc + 1], bias=lnp_sb[:, 1, dc:dc + 1]),
                         reads=["xt4", "lnp_sb"], writes=["x1b"])
                P.dma(fm(x1T)[:, :, ts_], x1b[:], reads=["x1b"], writes=["x1T"])
                ln_block(x1b[:], "x1b", mean, rstd, sq, "4")
                ln_apply(mdb[:], "mdb", x1b[:], "x1b", mean, rstd, "4")
                for dc in range(8):
                    P.op("act", lambda e: e.activation(out=mgb[:, dc, :], in_=mdb[:, dc, :], func=AF.Identity,
                                                       scale=s2p[:, dc:dc + 1], bias=adaT[:, SH2 + dc:SH2 + dc + 1]),
                         reads=["mdb", "s2p", "adaT"], writes=["mgb"])
                P.dma(fm(u2T)[:, :, ts_], mgb[:], reads=["mgb"], writes=["u2T"])
                for j in range(4):
                    for kc in range(8):
                        P.op("pe", lambda e: e.matmul(pb[4][:, j * 36:(j + 1) * 36], lhsT=mgb[:, kc, j * 128:(j + 1) * 128],
                                                      rhs=wr_sb[:, kc, :], start=(kc == 0), stop=False),
                             reads=["mgb", "wr_sb"], writes=[pbk[4]])
                    P.op("pe", lambda e: e.matmul(pb[4][:, j * 36:(j + 1) * 36], lhsT=ones[0:1, :], rhs=br_sb[0:1, :],
                                                  start=False, stop=True), reads=["ones", "br_sb"], writes=[pbk[4]])
                P.op("dve", lambda e: e.tensor_copy(out=L[:], in_=pb[4][:, 0:144].rearrange("p (j n) -> p j n", n=36)),
                     reads=[pbk[4]], writes=["L"])
                lg_ = L[:, :, 0:4]
                le_ = L[:, :, 4:36]
                gmax, gsum, wgrp, v1, v2, ex, w1, w2 = r4
                P.op("dve", lambda e: e.tensor_reduce(out=gmax[:], in_=lg_, axis=AX.X, op=ALU.max), reads=["L"], writes=["gmax"])
                P.op("dve", lambda e: e.tensor_tensor(out=g44[:], in0=lg_, in1=bc4(gmax, 4), op=ALU.subtract),
                     reads=["L", "gmax"], writes=["g44"])
                P.op("act", lambda e: e.activation(out=g44[:], in_=g44[:], func=AF.Exp), reads=["g44"], writes=["g44"])
                P.op("dve", lambda e: e.tensor_reduce(out=gsum[:], in_=g44[:], axis=AX.X, op=ALU.add), reads=["g44"], writes=["gsum"])
                P.op("dve", lambda e: e.reciprocal(out=wgrp[:], in_=gsum[:]), reads=["gsum"], writes=["wgrp"])
                P.op("dve", lambda e: e.tensor_tensor(out=g44[:], in0=lg_, in1=bc4(gmax, 4), op=ALU.is_equal),
                     reads=["L", "gmax", "g44"], writes=["g44"])
                P.op("dve", lambda e: e.tensor_scalar(out=g44[:], in0=g44[:], scalar1=-1.0, scalar2=BIG, op0=ALU.add, op1=ALU.mult),
                     reads=["g44"], writes=["g44"])
                P.op("dve", lambda e: e.tensor_tensor(out=lem[:].rearrange("p j (g k) -> p j g k", k=8),
                                                      in0=le_.rearrange("p j (g k) -> p j g k", k=8),
                                                      in1=g44[:].unsqueeze(3).to_broadcast([128, 4, 4, 8]), op=ALU.add),
                     reads=["L", "g44"], writes=["lem"])
                P.op("dve", lambda e: e.tensor_reduce(out=v1[:], in_=lem[:], axis=AX.X, op=ALU.max), reads=["lem"], writes=["v1"])
                P.op("dve", lambda e: e.tensor_tensor(out=m1[:], in0=lem[:], in1=bc4(v1, 32), op=ALU.is_equal),
                     reads=["lem", "v1"], writes=["m1"])
                P.op("dve", lambda e: e.scalar_tensor_tensor(out=lem2[:], in0=m1[:], scalar=-BIG, in1=lem[:], op0=ALU.mult, op1=ALU.add),
                     reads=["m1", "lem"], writes=["lem2"])
                P.op("dve", lambda e: e.tensor_reduce(out=v2[:], in_=lem2[:], axis=AX.X, op=ALU.max), reads=["lem2"], writes=["v2"])
                P.op("dve", lambda e: e.tensor_tensor(out=m2[:], in0=lem2[:], in1=bc4(v2, 32), op=ALU.is_equal),
                     reads=["lem2", "v2"], writes=["m2"])
                P.op("dve", lambda e: e.tensor_tensor(out=ex[:], in0=v2[:], in1=v1[:], op=ALU.subtract), reads=["v1", "v2"], writes=["ex"])
                P.op("act", lambda e: e.activation(out=ex[:], in_=ex[:], func=AF.Exp), reads=["ex"], writes=["ex"])
                P.op("dve", lambda e: e.tensor_scalar(out=w1[:], in0=ex[:], scalar1=1.0, scalar2=None, op0=ALU.add), reads=["ex"], writes=["w1"])
                P.op("dve", lambda e: e.reciprocal(out=w1[:], in_=w1[:]), reads=["w1"], writes=["w1"])
                P.op("dve", lambda e: e.tensor_tensor(out=w2[:], in0=ex[:], in1=w1[:], op=ALU.mult), reads=["ex", "w1"], writes=["w2"])
                P.op("dve", lambda e: e.tensor_tensor(out=w1[:], in0=w1[:], in1=wgrp[:], op=ALU.mult), reads=["w1", "wgrp"], writes=["w1"])
                P.op("dve", lambda e: e.tensor_tensor(out=w2[:], in0=w2[:], in1=wgrp[:], op=ALU.mult), reads=["w2", "wgrp"], writes=["w2"])
                P.op("dve", lambda e: e.tensor_tensor(out=m1[:], in0=m1[:], in1=bc4(w1, 32), op=ALU.mult), reads=["m1", "w1"], writes=["m1"])
                P.op("dve", lambda e: e.tensor_tensor(out=m2[:], in0=m2[:], in1=bc4(w2, 32), op=ALU.mult), reads=["m2", "w2"], writes=["m2"])
                P.op("dve", lambda e: e.tensor_tensor(out=Wts[:, tb * 4:(tb + 1) * 4, :], in0=m1[:], in1=m2[:], op=ALU.add),
                     reads=["m1", "m2"], writes=["Wts"])

    _sc.__exit__(None, None, None)
    if dbg == 1 and stage == 4:
        with ExitStack() as sd:
            tmp = sb(sd, "dbg_tmp4", [128, 8, 512])
            for tb in range(NB):
                P.dma(tmp[:], fm(u2T)[:, :, tb * 512:(tb + 1) * 512], reads=["u2T"], writes=["dbg_tmp4"])
                P.dma(fm(outT)[:, :, tb * 512:(tb + 1) * 512], tmp[:], reads=["dbg_tmp4"], writes=["outT"])

    P.barrier()
    _sc = nc.named_scope('st5'); _sc.__enter__()
    if stage >= 5:
        NTQ = TQ // 128
        NBQ = TQ // 512
        for q in range(NQ):
            with ExitStack() as sq5:
                qs = slice(q * TQ, (q + 1) * TQ)
                u2r = sb(sq5, "u2r", [128, 8, TQ], F32R)
                yacc = sb(sq5, "yacc", [128, NTQ, 1024])
                P.dma(u2r[:], fm(u2T)[:, :, qs], reads=["u2T"], writes=["u2r"], q="pool")
                with ExitStack() as se5:
                    gur = Ring([sb(se5, "wgu%d" % i, [128, 8, 1024], F32R) for i in range(2)], "wgu")
                    wdr = Ring([sb(se5, "wd%d" % i, [128, 4, 1024], F32R) for i in range(2)], "wd")
                    hring = Ring([sb(se5, "hT%d" % i, [128, 4, 512], F32R) for i in range(2)], "hT")
                    sgr = Ring([sb(se5, "sg%d" % i, [128, 512]) for i in range(2)], "sg")
                    cnt5 = {"it": 0, "yi": 0}
                    wcur = {}

                    def emit_gu(ex_, bq):
                        if bq == 0:
                            wg_, wgk = gur.next()
                            wd_, wdk = wdr.next()
                            P.dma(wg_[:], wgu[ex_], writes=[wgk], q="pool")
                            P.dma(wd_[:], wd[ex_], writes=[wdk], q="pool")
                            wcur[ex_] = (wg_, wgk, wd_, wdk)
                        wg_, wgk, wd_, wdk = wcur[ex_]
                        bs = slice(bq * 512, (bq + 1) * 512)
                        hT, hk = hring.next()
                        for fc in range(4):
                            it = cnt5["it"]
                            cnt5["it"] += 1
                            bg, bu = (it % 2) * 2, (it % 2) * 2 + 1
                            for kc in range(8):
                                P.op("pe", lambda e: e.matmul(pb[bg][:], lhsT=wg_[:, kc, fc * 128:(fc + 1) * 128], rhs=u2r[:, kc, bs],
                                                              start=(kc == 0), stop=(kc == 7)), reads=[wgk, "u2r"], writes=[pbk[bg]])
                            for kc in range(8):
                                P.op("pe", lambda e: e.matmul(pb[bu][:], lhsT=wg_[:, kc, 512 + fc * 128:512 + (fc + 1) * 128],
                                                              rhs=u2r[:, kc, bs], start=(kc == 0), stop=(kc == 7)),
                                     reads=[wgk, "u2r"], writes=[pbk[bu]])
                            sg_, sgk = sgr.next()
                            P.op("act", lambda e: e.activation(out=sg_[:], in_=pb[bg][:], func=AF.Silu), reads=[pbk[bg]], writes=[sgk])
                            P.op("dve", lambda e: e.tensor_tensor(out=hT[:, fc, :], in0=sg_[:], in1=pb[bu][:], op=ALU.mult),
                                 reads=[sgk, pbk[bu]], writes=[hk])
                        return hT, hk

                    def emit_y(ex_, bq, hT, hk):
                        wg_, wgk, wd_, wdk = wcur[ex_]
                        for tt in range(4):
                            tile = bq * 4 + tt
                            gt = q * NTQ + tile
                            for dh in range(2):
                                by = 4 + cnt5["yi"] % 2
                                cnt5["yi"] += 1
                                for fc in range(4):
                                    P.op("pe", lambda e: e.matmul(pb[by][:], lhsT=hT[:, fc, tt * 128:(tt + 1) * 128],
                                                                  rhs=wd_[:, fc, dh * 512:(dh + 1) * 512],
                                                                  start=(fc == 0), stop=(fc == 3)), reads=[hk, wdk], writes=[pbk[by]])
                                ya = yacc[:, tile, dh * 512:(dh + 1) * 512]
                                yk = "yacc%d_%d" % (tile, dh)
                                if ex_ == 0:
                                    P.op("dve", lambda e: e.tensor_scalar(out=ya, in0=pb[by][:], scalar1=Wts[:, gt, ex_:ex_ + 1],
                                                                          scalar2=None, op0=ALU.mult),
                                         reads=[pbk[by], "Wts"], writes=[yk])
                                else:
                                    P.op("dve", lambda e: e.scalar_tensor_tensor(out=ya, in0=pb[by][:], scalar=Wts[:, gt, ex_:ex_ + 1],
                                                                                  in1=ya, op0=ALU.mult, op1=ALU.add),
                                         reads=[pbk[by], "Wts", yk], writes=[yk])

                    blocks = [(ex_, bq) for ex_ in range(32) for bq in range(NBQ)]
                    if PIPE_MOE:
                        pend = emit_gu(*blocks[0])
                        for bi, (ex_, bq) in enumerate(blocks):
                            nxt = emit_gu(*blocks[bi + 1]) if bi + 1 < len(blocks) else None
                            emit_y(ex_, bq, *pend)
                            pend = nxt
                    else:
                        for (ex_, bq) in blocks:
                            emit_y(ex_, bq, *emit_gu(ex_, bq))
                P.barrier()
                with ExitStack() as sf5:
                    mT = sb(sf5, "mT", [128, 8, 512])
                    x1a = sb(sf5, "x1a", [128, 8, 512])
                    sq = sb(sf5, "sq5", [128, 8, 512], F32R)
                    mean = sb(sf5, "mean5", [128, 512])
                    rstd = sb(sf5, "rstd5", [128, 512])
                    for bq in range(NBQ):
                        gs = slice(q * TQ + bq * 512, q * TQ + (bq + 1) * 512)
                        P.dma(x1a[:], fm(x1T)[:, :, gs], reads=["x1T"], writes=["x1a"])
                        P.op("act", lambda e: e.mul(out=x1a[:], in_=x1a[:], mul=ALPHA), reads=["x1a"], writes=["x1a"])
                        for tt in range(4):
                            tile = bq * 4 + tt
                            for g4 in range(2):
                                bank = 6 + g4
                                for d4 in range(4):
                                    dc = g4 * 4 + d4
                                    P.op("pe", lambda e: e.transpose(pb[bank][:, d4 * 128:(d4 + 1) * 128],
                                                                     yacc[:, tile, dc * 128:(dc + 1) * 128], ident[:]),
                                         reads=["yacc%d_%d" % (tile, dc // 4), "ident"], writes=[pbk[bank]])
                                P.op("act", lambda e: e.copy(out=mT[:, g4 * 4:(g4 + 1) * 4, tt * 128:(tt + 1) * 128],
                                                             in_=pb[bank][:].rearrange("p (d t) -> p d t", t=128)),
                                     reads=[pbk[bank]], writes=["mT"])
                        for dc in range(8):
                            P.op("dve", lambda e: e.scalar_tensor_tensor(out=mT[:, dc, :], in0=mT[:, dc, :], scalar=adaT[:, G2 + dc:G2 + dc + 1],
                                                                          in1=x1a[:, dc, :], op0=ALU.mult, op1=ALU.add),
                                 reads=["mT", "adaT", "x1a"], writes=["mT"])
                        ln_block(mT[:], "mT", mean, rstd, sq, "5")
                        ln_apply(mT[:], "mT", mT[:], "mT", mean, rstd, "5")
                        for dc in range(8):
                            P.op("act", lambda e: e.activation(out=x1a[:, dc, :], in_=mT[:, dc, :], func=AF.Identity,
                                                               scale=lnp_sb[:, 2, dc:dc + 1], bias=lnp_sb[:, 3, dc:dc + 1]),
                                 reads=["mT", "lnp_sb"], writes=["x1a"])
                        P.dma(fm(outT)[:, :, gs], x1a[:], reads=["x1a"], writes=["outT"])
            P.barrier()

    _sc.__exit__(None, None, None)
    P.wait_all("sp", ["outT"])
    root.close()
    P.close()
    return nc, P


def _host_inputs(inp, S):
    f = np.float32
    B = inp["x"].shape[0]

    def pc(w):
        return np.ascontiguousarray(w.reshape(8, 128, -1).transpose(1, 0, 2))

    def col8(v):
        return np.ascontiguousarray(v.reshape(8, 128).T)

    w_in = inp["w_in"][0]
    gq, gk, gv, go, gr = w_in[:, 0:512], w_in[:, 512:1024], w_in[:, 1024:2048], w_in[:, 2048:3072], w_in[:, 3072:3088]
    dq, dk, dv = w_in[:, 3088:4112], w_in[:, 4112:5136], w_in[:, 5136:6160]
    ga, gb = w_in[:, 6160:7184], w_in[:, 7184:8208]
    wdiff = np.stack([pc(np.concatenate([dq[:, h * 128:(h + 1) * 128], dk[:, h * 128:(h + 1) * 128],
                                         dv[:, h * 128:(h + 1) * 128], gb[:, h * 128:(h + 1) * 128]], axis=1))
                      for h in range(8)])
    wgla = np.stack([pc(np.concatenate([gq[:, h * 128:(h + 1) * 128], gk[:, h * 128:(h + 1) * 128],
                                        gv[:, h * 256:(h + 1) * 256], go[:, h * 256:(h + 1) * 256],
                                        ga[:, h * 256:(h + 1) * 256]], axis=1)) for h in range(4)])
    inv = (10000.0 ** (-np.arange(32, dtype=f) / 32)).astype(f)
    p = np.arange(128)
    shared = {
        "invf": np.ascontiguousarray(inv[p % 32].reshape(128, 1)),
        "sgn": np.where((p % 64) < 32, -1.0, 1.0).astype(f).reshape(128, 1),
        "wada": pc(inp["w_ada"][0]),
        "bada": np.ascontiguousarray(inp["b_ada"][0].reshape(1, 6144)),
        "wdiff": np.ascontiguousarray(wdiff),
        "wgla": np.ascontiguousarray(wgla),
        "wgr": pc(gr),
        "wg2": np.ascontiguousarray(inp["w_gla_gate2"][0]),
        "bg2c": np.ascontiguousarray(inp["b_gla_gate2"][0].reshape(4, 128).T),
        "glanw": np.ascontiguousarray(np.broadcast_to(inp["gla_norm_w"][0][None, :], (128, 256))),
        "dnw": np.ascontiguousarray(inp["diff_norm_w"][0].reshape(128, 1)),
        "lamv": np.ascontiguousarray(np.broadcast_to(np.stack([inp["diff_lambda_q1"][0], inp["diff_lambda_k1"][0],
                                                               inp["diff_lambda_q2"][0], inp["diff_lambda_k2"][0]])[None],
                                                     (128, 4, 64))),
        "wout": pc(inp["w_out"][0]),
        "lnp": np.ascontiguousarray(np.stack([col8(inp["ln1_w"][0]), col8(inp["ln1_b"][0]),
                                              col8(inp["ln2_w"][0]), col8(inp["ln2_b"][0])], axis=1)),
        "wr": pc(np.concatenate([inp["w_router_group"][0], inp["w_router_expert"][0]], axis=1)),
        "br": np.ascontiguousarray(np.concatenate([inp["b_router_group"][0], inp["b_router_expert"][0]]).reshape(1, 36)),
        "wgu": np.ascontiguousarray(np.concatenate([inp["w_exp_gate"][0], inp["w_exp_up"][0]], axis=2)
                                    .reshape(32, 8, 128, 1024).transpose(0, 2, 1, 3)),
        "wd": np.ascontiguousarray(inp["w_exp_down"][0].reshape(32, 4, 128, 1024).transpose(0, 2, 1, 3)),
    }
    shared = {k: np.asarray(v, dtype=f) for k, v in shared.items()}
    maps = []
    for b in range(B):
        m = dict(shared)
        m["xT"] = np.ascontiguousarray(inp["x"][b, :S].T.astype(f))
        m["ccol"] = np.ascontiguousarray(inp["c"][b].reshape(8, 128).T.astype(f))
        m["posb"] = np.ascontiguousarray(np.broadcast_to(inp["positions"][b, :S].astype(np.int32)[None, :], (128, S)))
        maps.append(m)
    return maps


def kernel(**inputs):
    inp = {k: np.asarray(v) for k, v in inputs.items()}
    B, S, _ = inp["x"].shape
    nc, _ = build_program(S)
    maps = _host_inputs(inp, S)
    res = run_bass_kernel_spmd(nc, maps, core_ids=list(range(B)))
    out = np.stack([np.ascontiguousarray(res.results[b]["outT"].T) for b in range(B)])
    return out.astype(np.float32)
```

```python
import math
import numpy as np
import concourse.bass as bass
import concourse.mybir as mybir
from concourse.bass_utils import run_bass_kernel_spmd

F32 = mybir.dt.float32
F32R = mybir.dt.float32r
BF16 = mybir.dt.bfloat16
I32 = mybir.dt.int32
AF = mybir.ActivationFunctionType
ALU = mybir.AluOpType
AX = mybir.AxisListType

D = 1024
NCH = 8
LN_EPS = 1e-5
RMS_EPS = 1e-6
ALPHA = 2.0 ** 0.25
LAM_INIT = 0.2
TWO_PI = 2.0 * math.pi
CW1 = 6.28125
CW2 = TWO_PI - CW1
PI_LO = 3.1415925
MAGIC = 12582912.0
BIG = 1.0e4
PIPE_MOE = True


class Prog:
    def __init__(self, nc, n_dma_sems=32):
        self.nc = nc
        self.eng = {"pe": nc.tensor, "act": nc.scalar, "dve": nc.vector,
                    "pool": nc.gpsimd, "sp": nc.sync}
        self.sem, self.cnt, self.ctx = {}, {}, []
        for name in self.eng:
            cm = nc.semaphore("s_" + name)
            self.sem[name] = cm.__enter__()
            self.ctx.append(cm)
            self.cnt[name] = 0
        self.dma_sems = []
        for i in range(n_dma_sems):
            cm = nc.semaphore("s_dma%d" % i)
            self.dma_sems.append(cm.__enter__())
            self.ctx.append(cm)
        self.dma_cnt = [0] * n_dma_sems
        self.dma_rr = 0
        self.waited = {name: {} for name in self.eng}
        self.snap = {name: [None] for name in self.eng}
        self.snap_cur = {name: {} for name in self.eng}
        self.dma_snap = {}
        self.state = {}
        self.n_instr = 0

    def close(self):
        for cm in reversed(self.ctx):
            cm.__exit__(None, None, None)

    def _semobj(self, k):
        return self.sem[k] if isinstance(k, str) else self.dma_sems[k]

    def _wait(self, engname, token):
        k, val = token
        w = self.waited[engname]
        if w.get(k, 0) >= val:
            return
        self.eng[engname].wait_ge(self._semobj(k), val)
        w[k] = val
        self.n_instr += 1
        if isinstance(k, str):
            other = self.snap[k][val] if val < len(self.snap[k]) else None
        else:
            other = self.dma_snap.get((k, val))
        if other:
            changed = False
            for ok, ov in other.items():
                if ok == engname:
                    continue
                if w.get(ok, 0) < ov:
                    w[ok] = ov
                    changed = True
        self.snap_cur[engname] = None

    def _deps(self, engname, reads, writes):
        toks = {}

        def add(tok):
            if tok is None:
                return
            k, v = tok
            if toks.get(k, 0) < v:
                toks[k] = v
        for k in reads:
            st = self.state.get(k)
            if st is not None:
                add(st[0])
        for k in writes:
            st = self.state.get(k)
            if st is not None:
                add(st[0])
                for rk, rv in st[1].items():
                    add((rk, rv))
        for k, v in toks.items():
            if engname == "pe" and k == "pe":
                continue
            self._wait(engname, (k, v))

    def _commit(self, token, reads, writes):
        for k in writes:
            self.state[k] = [token, {}]
        sk, sv = token
        for k in reads:
            st = self.state.get(k)
            if st is None:
                st = self.state[k] = [None, {}]
            if st[1].get(sk, 0) < sv:
                st[1][sk] = sv

    def op(self, engname, fn, reads=(), writes=()):
        self._deps(engname, reads, writes)
        ins = fn(self.eng[engname])
        self.cnt[engname] += 1
        if self.snap_cur[engname] is None:
            self.snap_cur[engname] = dict(self.waited[engname])
        self.snap[engname].append(self.snap_cur[engname])
        ins.then_inc(self.sem[engname], 1)
        self._commit((engname, self.cnt[engname]), reads, writes)
        self.n_instr += 1
        return ins

    def dma(self, out, in_, reads=(), writes=(), q="sp", **kw):
        self._deps(q, reads, writes)
        j = self.dma_rr
        self.dma_rr = (self.dma_rr + 1) % len(self.dma_sems)
        if self.dma_cnt[j] > 0:
            self._wait(q, (j, 16 * self.dma_cnt[j]))
        ins = self.eng[q].dma_start(out=out, in_=in_, **kw)
        self.dma_cnt[j] += 1
        ins.then_inc(self.dma_sems[j], 16)
        self.dma_snap[(j, 16 * self.dma_cnt[j])] = dict(self.waited[q])
        self._commit((j, 16 * self.dma_cnt[j]), reads, writes)
        self.n_instr += 1

    def barrier(self):
        for e in self.eng:
            for x in self.eng:
                if x != e and self.cnt[x] > 0:
                    self._wait(e, (x, self.cnt[x]))
            for j in range(len(self.dma_sems)):
                if self.dma_cnt[j] > 0:
                    self._wait(e, (j, 16 * self.dma_cnt[j]))
        self.state = {}

    def wait_all(self, engname, keys):
        for k in keys:
            st = self.state.get(k)
            if st is not None and st[0] is not None:
                self._wait(engname, st[0])


class Ring:
    def __init__(self, tiles, name):
        self.tiles = tiles
        self.name = name
        self.i = 0

    def next(self):
        j = self.i % len(self.tiles)
        self.i += 1
        return self.tiles[j], "%s%d" % (self.name, j)


def build_program(S, stage=99, dbg=False):
    NB = S // 512
    NT = S // 128
    TQ = min(1024, S)
    NQ = S // TQ
    nc = bass.Bass("TRN2", target_bir_lowering=False)

    def din(name, shape, dt=F32):
        return nc.dram_tensor(name, list(shape), dt, kind="ExternalInput").ap()

    xT = din("xT", [D, S])
    ccol = din("ccol", [128, 8])
    posb = din("posb", [128, S], I32)
    invf = din("invf", [128, 1])
    sgn = din("sgn", [128, 1])
    wada = din("wada", [128, 8, 6144])
    bada = din("bada", [1, 6144])
    wdiff = din("wdiff", [8, 128, 8, 512])
    wgla = din("wgla", [4, 128, 8, 1024])
    wgr = din("wgr", [128, 8, 16])
    wg2 = din("wg2", [16, 512])
    bg2c = din("bg2c", [128, 4])
    glanw = din("glanw", [128, 256])
    dnw = din("dnw", [128, 1])
    lamv = din("lamv", [128, 4, 64])
    wout = din("wout", [128, 8, 1024])
    lnp = din("lnp", [128, 4, 8])
    wr = din("wr", [128, 8, 36])
    br = din("br", [1, 36])
    if stage >= 5:
        wgu = din("wgu", [32, 128, 8, 1024])
        wd = din("wd", [32, 128, 4, 1024])
    outT = nc.dram_tensor("outT", [D, S], F32, kind="ExternalOutput").ap()
    mixd = nc.dram_tensor("mixd", [D, S], F32, kind="Internal").ap()
    mixg = nc.dram_tensor("mixg", [D, S], F32, kind="Internal").ap()
    x1T = nc.dram_tensor("x1T", [D, S], F32, kind="Internal").ap()
    u2T = nc.dram_tensor("u2T", [D, S], F32, kind="Internal").ap()

    def fm(ap):
        return ap.rearrange("(c p) t -> p c t", p=128)

    P = Prog(nc)
    from contextlib import ExitStack
    root = ExitStack()

    used_names = {}

    def sb(stack, name, shape, dt=F32):
        n = used_names.get(name, 0)
        used_names[name] = n + 1
        if n:
            name = "%s_r%d" % (name, n)
        return stack.enter_context(nc.sbuf_tensor(name, list(shape), dt))

    pb = [root.enter_context(nc.psum_tensor("pb%d" % i, [128, 512], F32)) for i in range(8)]
    pbk = ["pb%d" % i for i in range(8)]

    ident = sb(root, "ident", [128, 128])
    ones = sb(root, "ones", [128, 128])
    ones_r = sb(root, "ones_r", [128, 128], F32R)
    ident_r = sb(root, "ident_r", [128, 128], F32R)
    adaT = sb(root, "adaT", [128, 48])
    s1p = sb(root, "s1p", [128, 8])
    s2p = sb(root, "s2p", [128, 8])
    lnp_sb = sb(root, "lnp_sb", [128, 4, 8])
    Wts = sb(root, "Wts", [128, NT, 32])
    junk = sb(root, "junk", [128, 512])

    P.op("pool", lambda e: e.memset(ones[:], 1.0), writes=["ones"])
    P.op("pool", lambda e: e.memset(ident[:], 1.0), writes=["ident"])
    P.op("pool", lambda e: e.affine_select(out=ident[:], in_=ident[:], pattern=[[-1, 128]],
                                           compare_op=ALU.is_equal, fill=0.0, base=0,
                                           channel_multiplier=1), reads=["ident"], writes=["ident"])
    P.op("dve", lambda e: e.tensor_copy(out=ones_r[:], in_=ones[:]), reads=["ones"], writes=["ones_r"])
    P.op("dve", lambda e: e.tensor_copy(out=ident_r[:], in_=ident[:]), reads=["ident"], writes=["ident_r"])
    P.dma(lnp_sb[:], lnp, writes=["lnp_sb"])

    def ln_block(src, src_key, mean, rstd, sq, tag):
        P.op("act", lambda e: e.activation(out=sq[:], in_=src, func=AF.Square), reads=[src_key], writes=["sq"])
        for c in range(8):
            P.op("pe", lambda e: e.matmul(pb[0][:], lhsT=ones[:], rhs=src[:, c, :], start=(c == 0), stop=(c == 7)),
                 reads=["ones", src_key], writes=[pbk[0]])
        for c in range(8):
            P.op("pe", lambda e: e.matmul(pb[1][:], lhsT=ones_r[:], rhs=sq[:, c, :], start=(c == 0), stop=(c == 7)),
                 reads=["ones_r", "sq"], writes=[pbk[1]])
        P.op("dve", lambda e: e.tensor_scalar(out=mean[:], in0=pb[0][:], scalar1=1.0 / D, scalar2=None, op0=ALU.mult),
             reads=[pbk[0]], writes=["mean" + tag])
        P.op("dve", lambda e: e.tensor_tensor(out=rstd[:], in0=mean[:], in1=mean[:], op=ALU.mult),
             reads=["mean" + tag], writes=["rstd" + tag])
        P.op("dve", lambda e: e.scalar_tensor_tensor(out=rstd[:], in0=pb[1][:], scalar=1.0 / D, in1=rstd[:],
                                                      op0=ALU.mult, op1=ALU.subtract),
             reads=[pbk[1], "rstd" + tag], writes=["rstd" + tag])
        P.op("act", lambda e: e.activation(out=rstd[:], in_=rstd[:], func=AF.Sqrt, bias=LN_EPS),
             reads=["rstd" + tag], writes=["rstd" + tag])
        P.op("dve", lambda e: e.reciprocal(out=rstd[:], in_=rstd[:]), reads=["rstd" + tag], writes=["rstd" + tag])

    def ln_apply(dst, dst_key, src, src_key, mean, rstd, tag):
        mb = mean[:].unsqueeze(1).to_broadcast([128, 8, 512])
        rb = rstd[:].unsqueeze(1).to_broadcast([128, 8, 512])
        P.op("dve", lambda e: e.tensor_tensor(out=dst, in0=src, in1=mb, op=ALU.subtract),
             reads=[src_key, "mean" + tag], writes=[dst_key])
        P.op("dve", lambda e: e.tensor_tensor(out=dst, in0=dst, in1=rb, op=ALU.mult),
             reads=[dst_key, "rstd" + tag], writes=[dst_key])

    _sc = nc.named_scope('st0'); _sc.__enter__()
    with ExitStack() as st0:
        cc = sb(st0, "cc", [128, 8])
        scb = sb(st0, "scb", [128, 8, 128])
        bada_sb = sb(st0, "bada_sb", [1, 6144])
        ada_bc = sb(st0, "ada_bc", [128, 6144])
        wring = Ring([sb(st0, "wada%d" % i, [128, 8, 512]) for i in range(2)], "wada")
        P.dma(cc[:], ccol, writes=["cc"])
        P.dma(bada_sb[:], bada, writes=["bada_sb"])
        P.op("act", lambda e: e.activation(out=cc[:], in_=cc[:], func=AF.Silu), reads=["cc"], writes=["cc"])
        for kc in range(8):
            P.op("dve", lambda e: e.tensor_copy(out=scb[:, kc, :], in_=cc[:, kc:kc + 1].to_broadcast([128, 128])),
                 reads=["cc"], writes=["scb"])
        for cg in range(12):
            wt, wk = wring.next()
            P.dma(wt[:], wada[:, :, cg * 512:(cg + 1) * 512], writes=[wk])
            bank = cg % 2
            for kc in range(8):
                P.op("pe", lambda e: e.matmul(pb[bank][:], lhsT=scb[:, kc, :], rhs=wt[:, kc, :], start=(kc == 0), stop=False),
                     reads=["scb", wk], writes=[pbk[bank]])
            P.op("pe", lambda e: e.matmul(pb[bank][:], lhsT=ones[0:1, :], rhs=bada_sb[0:1, cg * 512:(cg + 1) * 512],
                                          start=False, stop=True), reads=["ones", "bada_sb"], writes=[pbk[bank]])
            P.op("act", lambda e: e.copy(out=ada_bc[:, cg * 512:(cg + 1) * 512], in_=pb[bank][:]),
                 reads=[pbk[bank]], writes=["ada_bc"])
        for j in range(48):
            P.op("dve", lambda e: e.scalar_tensor_tensor(out=junk[:, 0:128], in0=ada_bc[:, j * 128:(j + 1) * 128], scalar=1.0,
                                                          in1=ident[:], op0=ALU.mult, op1=ALU.mult,
                                                          accum_out=adaT[:, j:j + 1]),
                 reads=["ada_bc", "ident"], writes=["junk", "adaT"])
        P.op("dve", lambda e: e.tensor_scalar(out=s1p[:], in0=adaT[:, 8:16], scalar1=1.0, scalar2=None, op0=ALU.add),
             reads=["adaT"], writes=["s1p"])
        P.op("dve", lambda e: e.tensor_scalar(out=s2p[:], in0=adaT[:, 32:40], scalar1=1.0, scalar2=None, op0=ALU.add),
             reads=["adaT"], writes=["s2p"])
    _sc.__exit__(None, None, None)
    SH1, G1, SH2, G2 = 0, 16, 24, 40
    P.barrier()

    mix_stack = ExitStack()
    uT = sb(mix_stack, "uT", [128, 8, S], BF16)

    _sc = nc.named_scope('st1'); _sc.__enter__()
    with ExitStack() as st1:
        xring = Ring([sb(st1, "xt%d" % i, [128, 8, 512]) for i in range(2)], "xt")
        sq = sb(st1, "sq", [128, 8, 512], F32R)
        mean = sb(st1, "mean", [128, 512])
        rstd = sb(st1, "rstd", [128, 512])
        for tb in range(NB):
            xt, xk = xring.next()
            P.dma(xt[:], fm(xT)[:, :, tb * 512:(tb + 1) * 512], writes=[xk])
            ln_block(xt[:], xk, mean, rstd, sq, "1")
            ln_apply(xt[:], xk, xt[:], xk, mean, rstd, "1")
            for c in range(8):
                P.op("act", lambda e: e.activation(out=uT[:, c, tb * 512:(tb + 1) * 512], in_=xt[:, c, :], func=AF.Identity,
                                                   scale=s1p[:, c:c + 1], bias=adaT[:, SH1 + c:SH1 + c + 1]),
                     reads=[xk, "s1p", "adaT"], writes=["uT%d" % tb])

    _sc.__exit__(None, None, None)
    P.barrier()
    if dbg and stage == 1:
        with ExitStack() as sd:
            tmp = sb(sd, "dbg_tmp", [128, 8, 512])
            for tb in range(NB):
                P.op("dve", lambda e: e.tensor_copy(out=tmp[:], in_=uT[:, :, tb * 512:(tb + 1) * 512]),
                     reads=["uT%d" % tb], writes=["dbg_tmp"])
                P.dma(fm(outT)[:, :, tb * 512:(tb + 1) * 512], tmp[:], reads=["dbg_tmp"], writes=["outT"])

    _sc = nc.named_scope('st2'); _sc.__enter__()
    if stage >= 2:
        with ExitStack() as st2:
            cosT = sb(st2, "cosT", [128, S])
            sinS = sb(st2, "sinS", [128, S])
            invf_sb = sb(st2, "invf_sb", [128, 1])
            sgn_sb = sb(st2, "sgn_sb", [128, 1])
            dnws = sb(st2, "dnws", [128, 1])
            neglam = sb(st2, "neglam", [128, 1])
            perm_r = sb(st2, "perm_r", [128, 128], F32R)
            maskA = sb(st2, "maskA", [128, 512])
            maskB = sb(st2, "maskB", [128, 512])
            P.dma(invf_sb[:], invf, writes=["invf_sb"])
            P.dma(sgn_sb[:], sgn, writes=["sgn_sb"])
            P.dma(dnws[:], dnw, writes=["dnws"])
            P.op("dve", lambda e: e.tensor_scalar(out=dnws[:], in0=dnws[:], scalar1=1.0 - LAM_INIT, scalar2=None, op0=ALU.mult),
                 reads=["dnws"], writes=["dnws"])
            for (d0, s0) in ((0, 32), (32, 0), (64, 96), (96, 64)):
                P.op("dve", lambda e: e.tensor_copy(out=perm_r[:, d0:d0 + 32], in_=ident[:, s0:s0 + 32]),
                     reads=["ident"], writes=["perm_r"])
            for (mt, mk, base) in ((maskA, "maskA", 0), (maskB, "maskB", -128)):
                P.op("pool", lambda e: e.memset(mt[:], 1.0), writes=[mk])
                for c in range(2):
                    P.op("pool", lambda e: e.affine_select(out=mt[:, c * 256:(c + 1) * 256], in_=mt[:, c * 256:(c + 1) * 256],
                                                           pattern=[[1, 256]], compare_op=ALU.is_ge, fill=0.0,
                                                           base=base, channel_multiplier=-1), reads=[mk], writes=[mk])
            with ExitStack() as sl:
                lv = sb(sl, "lv", [128, 4, 64])
                lacc = sb(sl, "lacc", [128, 2])
                P.dma(lv[:], lamv, writes=["lv"])
                for i in range(2):
                    P.op("dve", lambda e: e.scalar_tensor_tensor(out=junk[:, 0:64], in0=lv[:, 2 * i, :], scalar=1.0, in1=lv[:, 2 * i + 1, :],
                                                                  op0=ALU.mult, op1=ALU.mult, accum_out=lacc[:, i:i + 1]),
                         reads=["lv"], writes=["junk", "lacc"])
                P.op("act", lambda e: e.activation(out=lacc[:], in_=lacc[:], func=AF.Exp), reads=["lacc"], writes=["lacc"])
                P.op("dve", lambda e: e.tensor_tensor(out=neglam[:], in0=lacc[:, 1:2], in1=lacc[:, 0:1], op=ALU.subtract),
                     reads=["lacc"], writes=["neglam"])
                P.op("dve", lambda e: e.tensor_scalar(out=neglam[:], in0=neglam[:], scalar1=-LAM_INIT, scalar2=None, op0=ALU.add),
                     reads=["neglam"], writes=["neglam"])
            P.barrier()
            with ExitStack() as sr:
                posi = sb(sr, "posi", [128, S], I32)
                ang = sb(sr, "ang", [128, S])
                kf = sb(sr, "kf", [128, S])
                P.dma(posi[:], posb, writes=["posi"])
                P.op("dve", lambda e: e.tensor_copy(out=ang[:], in_=posi[:]), reads=["posi"], writes=["ang"])
                P.op("dve", lambda e: e.tensor_scalar(out=ang[:], in0=ang[:], scalar1=invf_sb[:, 0:1], scalar2=None, op0=ALU.mult),
                     reads=["ang", "invf_sb"], writes=["ang"])
                for (dst, dk_, off, post) in ((sinS, "sinS", 0.0, 0.0), (cosT, "cosT", 0.25, math.pi / 2)):
                    P.op("dve", lambda e: e.tensor_scalar(out=kf[:], in0=ang[:], scalar1=1.0 / TWO_PI, scalar2=off,
                                                          op0=ALU.mult, op1=ALU.add), reads=["ang"], writes=["kf"])
                    P.op("dve", lambda e: e.tensor_scalar(out=kf[:], in0=kf[:], scalar1=MAGIC, scalar2=None, op0=ALU.add),
                         reads=["kf"], writes=["kf"])
                    P.op("dve", lambda e: e.tensor_scalar(out=kf[:], in0=kf[:], scalar1=-MAGIC, scalar2=None, op0=ALU.add),
                         reads=["kf"], writes=["kf"])
                    P.op("dve", lambda e: e.scalar_tensor_tensor(out=dst[:], in0=kf[:], scalar=-CW1, in1=ang[:],
                                                                  op0=ALU.mult, op1=ALU.add), reads=["kf", "ang"], writes=[dk_])
                    P.op("dve", lambda e: e.scalar_tensor_tensor(out=dst[:], in0=kf[:], scalar=-CW2, in1=dst[:],
                                                                  op0=ALU.mult, op1=ALU.add), reads=["kf", dk_], writes=[dk_])
                    P.op("dve", lambda e: e.tensor_scalar(out=dst[:], in0=dst[:], scalar1=post, scalar2=PI_LO,
                                                          op0=ALU.add, op1=ALU.min), reads=[dk_], writes=[dk_])
                    P.op("dve", lambda e: e.tensor_scalar(out=dst[:], in0=dst[:], scalar1=-PI_LO, scalar2=None, op0=ALU.max),
                         reads=[dk_], writes=[dk_])
                    P.op("act", lambda e: e.activation(out=dst[:], in_=dst[:], func=AF.Sin), reads=[dk_], writes=[dk_])
                P.op("dve", lambda e: e.tensor_scalar(out=sinS[:], in0=sinS[:], scalar1=sgn_sb[:, 0:1], scalar2=None, op0=ALU.mult),
                     reads=["sinS", "sgn_sb"], writes=["sinS"])

            P.barrier()
            wbfs = [sb(st2, "wbf%d" % i, [128, 8, 512], BF16) for i in range(2)]
            kT = sb(st2, "kT", [128, S], F32R)
            vtok = sb(st2, "vtok", [128, NT, 128], F32R)
            qexps = [sb(st2, "qexp%d" % i, [128, 2, 512], F32R) for i in range(2)]
            gbTs = [sb(st2, "gbT%d" % i, [128, 512]) for i in range(2)]
            rawr = Ring([sb(st2, "raw%d" % i, [128, 512], F32R) for i in range(2)], "raw")
            t1r = Ring([sb(st2, "t1_%d" % i, [128, 512]) for i in range(2)], "t1_")
            t2r = Ring([sb(st2, "t2_%d" % i, [128, 512]) for i in range(2)], "t2_")
            vTs = sb(st2, "vTs", [128, 512])
            pring = Ring([sb(st2, "pT%d" % i, [128, 512], F32R) for i in range(5)], "pT")
            rz = sb(st2, "rz", [128, 512])
            on = sb(st2, "on", [128, 512])
            od = sb(st2, "od", [128, 256])
            osq = sb(st2, "osq", [128, 256], F32R)
            rn = sb(st2, "rn", [128, 256])
            mo = sb(st2, "mo", [128, 256])
            for i in range(2):
                P.op("pool", lambda e: e.memset(qexps[i][:].bitcast(F32), 0.0), writes=["qexp%d" % i])
            PJ = (0, 1)
            PP, PO, PZ = 2, 5, 6
            PN = PP
            sring = Ring([pb[3], pb[4], pb[7]], "psS")
            LOOK = 2
            PVDELAY = 1

            def load_w(h):
                P.dma(wbfs[h % 2][:], wdiff[h], writes=["wbf%d" % (h % 2)], q="pool")

            def proj_gen(n, h, tb):
                p = n % 2
                wbf, wk = wbfs[h % 2], "wbf%d" % (h % 2)
                qexp, qk = qexps[p], "qexp%d" % p
                gbT, gk = gbTs[p], "gbT%d" % p
                ts_ = slice(tb * 512, (tb + 1) * 512)
                uk = "uT%d" % tb
                for which in (1, 0):
                    bank = PJ[which]
                    for kc in range(8):
                        P.op("pe", lambda e: e.matmul(pb[bank][:], lhsT=wbf[:, kc, which * 128:(which + 1) * 128],
                                                      rhs=uT[:, kc, ts_], start=(kc == 0), stop=(kc == 7)),
                             reads=[wk, uk], writes=[pbk[bank]])
                    yield
                    raw, rk = rawr.next()
                    t1, t1k = t1r.next()
                    t2, t2k = t2r.next()
                    P.op("act", lambda e: e.activation(out=raw[:], in_=pb[bank][:], func=AF.Copy,
                                                       scale=(0.125 if which == 0 else 1.0)),
                         reads=[pbk[bank]], writes=[rk])
                    P.op("pe", lambda e: e.matmul(pb[PP][:], lhsT=perm_r[:], rhs=raw[:], start=True, stop=True),
                         reads=["perm_r", rk], writes=[pbk[PP]])
                    P.op("pool", lambda e: e.tensor_tensor(out=t1[:], in0=raw[:].bitcast(F32), in1=cosT[:, ts_], op=ALU.mult),
                         reads=[rk, "cosT"], writes=[t1k])
                    P.op("dve", lambda e: e.tensor_tensor(out=t2[:], in0=pb[PP][:], in1=sinS[:, ts_], op=ALU.mult),
                         reads=[pbk[PP], "sinS"], writes=[t2k])
                    if which == 0:
                        for c in range(2):
                            ps_ = slice(c * 64, (c + 1) * 64)
                            P.op("dve", lambda e: e.tensor_tensor(
                                out=qexp[ps_, :, c * 256:(c + 1) * 256],
                                in0=t1[ps_, :].rearrange("p (j q) -> p j q", q=256),
                                in1=t2[ps_, :].rearrange("p (j q) -> p j q", q=256), op=ALU.add),
                                reads=[t1k, t2k], writes=[qk])
                    else:
                        P.op("dve", lambda e: e.tensor_tensor(out=kT[:, ts_], in0=t1[:], in1=t2[:], op=ALU.add),
                             reads=[t1k, t2k], writes=["kT%d" % tb])
                    yield
                for kc in range(8):
                    P.op("pe", lambda e: e.matmul(pb[PJ[0]][:], lhsT=wbf[:, kc, 256:384], rhs=uT[:, kc, ts_],
                                                  start=(kc == 0), stop=(kc == 7)), reads=[wk, uk], writes=[pbk[PJ[0]]])
                yield
                P.op("act", lambda e: e.copy(out=vTs[:], in_=pb[PJ[0]][:]), reads=[pbk[PJ[0]]], writes=["vTs"])
                for kc in range(8):
                    P.op("pe", lambda e: e.matmul(pb[PJ[1]][:], lhsT=wbf[:, kc, 384:512], rhs=uT[:, kc, ts_],
                                                  start=(kc == 0), stop=(kc == 7)), reads=[wk, uk], writes=[pbk[PJ[1]]])
                P.op("act", lambda e: e.activation(out=gbT[:], in_=pb[PJ[1]][:], func=AF.Sigmoid),
                     reads=[pbk[PJ[1]]], writes=[gk])
                yield
                for j in range(4):
                    P.op("pe", lambda e: e.transpose(pb[PP][:, j * 128:(j + 1) * 128], vTs[:, j * 128:(j + 1) * 128], ident[:]),
                         reads=["vTs", "ident"], writes=[pbk[PP]])
                P.op("act", lambda e: e.copy(out=vtok[:, tb * 4:(tb + 1) * 4, :],
                                             in_=pb[PP][:].rearrange("p (j d) -> p j d", d=128)),
                     reads=[pbk[PP]], writes=["vtok%d" % tb])
                yield

            def attn_gen(n, h, tb):
                p = n % 2
                qexp, qk = qexps[p], "qexp%d" % p
                gbT, gk = gbTs[p], "gbT%d" % p
                for jl in range(2):
                    jb = tb * 2 + jl
                    nk = 2 * jb + 2
                    sbanks = {}

                    def qk_mm(i):
                        sps, spk = sring.next()
                        sbanks[i] = (sps, spk)
                        P.op("pe", lambda e: e.matmul(sps[:], lhsT=kT[:, i * 128:(i + 1) * 128], rhs=qexp[:, jl, :],
                                                      start=True, stop=True),
                             reads=["kT%d" % (i // 4), qk], writes=[spk])
                    pend_pv = []

                    def pv_mm(i, pt, ptk):
                        P.op("pe", lambda e: e.matmul(pb[PO][:], lhsT=vtok[:, i, :], rhs=pt[:], start=(i == 0), stop=(i == nk - 1)),
                             reads=["vtok%d" % (i // 4), ptk], writes=[pbk[PO]])
                        P.op("pe", lambda e: e.matmul(pb[PZ][:], lhsT=ones_r[:], rhs=pt[:], start=(i == 0), stop=(i == nk - 1)),
                             reads=["ones_r", ptk], writes=[pbk[PZ]])
                    for i in range(min(LOOK, nk)):
                        qk_mm(i)
                    for i in range(nk):
                        if i + LOOK < nk:
                            qk_mm(i + LOOK)
                        sps, spk = sbanks.pop(i)
                        pt, ptk = pring.next()
                        P.op("act", lambda e: e.activation(out=pt[:], in_=sps[:], func=AF.Exp), reads=[spk], writes=[ptk])
                        if i >= 2 * jb:
                            mt, mk = (maskA, "maskA") if i == 2 * jb else (maskB, "maskB")
                            P.op("dve", lambda e: e.tensor_tensor(out=pt[:], in0=pt[:].bitcast(F32), in1=mt[:], op=ALU.mult),
                                 reads=[ptk, mk], writes=[ptk])
                        pend_pv.append((i, pt, ptk))
                        if len(pend_pv) > PVDELAY:
                            pv_mm(*pend_pv.pop(0))
                        yield
                    while pend_pv:
                        pv_mm(*pend_pv.pop(0))
                    P.op("dve", lambda e: e.reciprocal(out=rz[:], in_=pb[PZ][:]), reads=[pbk[PZ]], writes=["rz"])
                    P.op("dve", lambda e: e.tensor_tensor(out=on[:], in0=pb[PO][:], in1=rz[:], op=ALU.mult),
                         reads=[pbk[PO], "rz"], writes=["on"])
                    P.op("dve", lambda e: e.scalar_tensor_tensor(out=od[:], in0=on[:, 256:512], scalar=neglam[:, 0:1],
                                                                  in1=on[:, 0:256], op0=ALU.mult, op1=ALU.add),
                         reads=["on", "neglam"], writes=["od"])
                    P.op("act", lambda e: e.activation(out=osq[:], in_=od[:], func=AF.Square), reads=["od"], writes=["osq"])
                    P.op("pe", lambda e: e.matmul(pb[PN][:, 0:256], lhsT=ones_r[:], rhs=osq[:], start=True, stop=True),
                         reads=["ones_r", "osq"], writes=[pbk[PN]])
                    P.op("act", lambda e: e.activation(out=rn[:], in_=pb[PN][:, 0:256], func=AF.Sqrt, scale=1.0 / 128, bias=RMS_EPS),
                         reads=[pbk[PN]], writes=["rn"])
                    P.op("dve", lambda e: e.reciprocal(out=rn[:], in_=rn[:]), reads=["rn"], writes=["rn"])
                    P.op("dve", lambda e: e.scalar_tensor_tensor(out=mo[:], in0=od[:], scalar=dnws[:, 0:1], in1=rn[:],
                                                                  op0=ALU.mult, op1=ALU.mult),
                         reads=["od", "dnws", "rn"], writes=["mo"])
                    P.op("dve", lambda e: e.tensor_tensor(out=mo[:], in0=mo[:], in1=gbT[:, jl * 256:(jl + 1) * 256], op=ALU.mult),
                         reads=["mo", gk], writes=["mo"])
                    P.dma(mixd[h * 128:(h + 1) * 128, jb * 256:(jb + 1) * 256], mo[:], reads=["mo"], writes=["mixd"])
                    yield

            def run_all(g):
                for _ in g:
                    pass

            def interleave(ga, gb_):
                gens = [ga, gb_]
                while gens:
                    for g in list(gens):
                        try:
                            next(g)
                        except StopIteration:
                            gens.remove(g)

            items = [(h, tb) for h in range(8) for tb in range(NB)]
            load_w(0)
            if len(items) > 0:
                load_w(1)
            run_all(proj_gen(0, 0, 0))
            for n, (h, tb) in enumerate(items):
                nxt = items[n + 1] if n + 1 < len(items) else None
                if nxt is not None and nxt[0] == h:
                    interleave(attn_gen(n, h, tb), proj_gen(n + 1, nxt[0], nxt[1]))
                else:
                    run_all(attn_gen(n, h, tb))
                    if h + 2 < 8:
                        load_w(h + 2)
                    if nxt is not None:
                        run_all(proj_gen(n + 1, nxt[0], nxt[1]))

    _sc.__exit__(None, None, None)
    if dbg == 1 and stage == 2:
        with ExitStack() as sd:
            tmp = sb(sd, "dbg_tmp2", [128, 8, 512])
            for tb in range(NB):
                P.dma(tmp[:], fm(mixd)[:, :, tb * 512:(tb + 1) * 512], reads=["mixd"], writes=["dbg_tmp2"])
                P.dma(fm(outT)[:, :, tb * 512:(tb + 1) * 512], tmp[:], reads=["dbg_tmp2"], writes=["outT"])


    P.barrier()
    _sc = nc.named_scope('st3'); _sc.__enter__()
    if stage >= 3:
        with ExitStack() as st3:
            cmask = sb(st3, "cmask", [128, 512])
            mask2 = sb(st3, "mask2", [128, 128])
            wg2_sb = sb(st3, "wg2_sb", [16, 512])
            negb = sb(st3, "negb", [128, 4])
            glanw_sb = sb(st3, "glanw_sb", [128, 256])
            wgr_st = sb(st3, "wgr_st", [128, 8, 16])
            wgr_bf = sb(st3, "wgr_bf", [128, 8, 16], BF16)
            glrT = sb(st3, "glrT", [16, S])
            P.op("pool", lambda e: e.memset(cmask[:], 1.0), writes=["cmask"])
            P.op("pool", lambda e: e.memset(cmask[:].rearrange("p (n c) -> p n c", c=64)[:, :, 0:1], 0.0),
                 reads=["cmask"], writes=["cmask"])
            P.op("pool", lambda e: e.memset(mask2[:], 1.0), writes=["mask2"])
            P.op("pool", lambda e: e.affine_select(out=mask2[:], in_=mask2[:], pattern=[[1, 128]], compare_op=ALU.is_ge,
                                                   fill=0.0, base=0, channel_multiplier=-1), reads=["mask2"], writes=["mask2"])
            P.op("pool", lambda e: e.memset(mask2[0:64, 64:128], 0.0), reads=["mask2"], writes=["mask2"])
            P.dma(wg2_sb[:], wg2, writes=["wg2_sb"])
            P.dma(negb[:], bg2c, writes=["negb"])
            P.op("dve", lambda e: e.tensor_scalar(out=negb[:], in0=negb[:], scalar1=-1.0, scalar2=None, op0=ALU.mult),
                 reads=["negb"], writes=["negb"])
            P.dma(glanw_sb[:], glanw, writes=["glanw_sb"])
            P.dma(wgr_st[:], wgr, writes=["wgr_st"])
            P.op("dve", lambda e: e.tensor_copy(out=wgr_bf[:], in_=wgr_st[:]), reads=["wgr_st"], writes=["wgr_bf"])
            for tb in range(NB):
                ts_ = slice(tb * 512, (tb + 1) * 512)
                for kc in range(8):
                    P.op("pe", lambda e: e.matmul(pb[0][0:16, :], lhsT=wgr_bf[:, kc, :], rhs=uT[:, kc, ts_],
                                                  start=(kc == 0), stop=(kc == 7)), reads=["wgr_bf", "uT%d" % tb], writes=[pbk[0]])
                P.op("act", lambda e: e.copy(out=glrT[:, ts_], in_=pb[0][0:16, :]), reads=[pbk[0]], writes=["glrT%d" % tb])

            def mkslot(sl):
                B = {}
                B["wbf2"] = sb(st3, "wbf2_%d" % sl, [128, 8, 1024], BF16)
                B["st"] = [sb(st3, "stA_%d" % sl, [128, 256]), sb(st3, "stB_%d" % sl, [128, 256])]
                for nm in ("spl", "cs", "eb", "enb", "ed", "qt", "kt", "kd"):
                    B[nm] = sb(st3, "%s_%d" % (nm, sl), [128, 512])
                B["dec"] = sb(st3, "dec_%d" % sl, [128, 8])
                B["kdt"] = sb(st3, "kdt_%d" % sl, [128, 4, 128])
                for nm in ("vt", "sog", "sga"):
                    B[nm] = sb(st3, "%s_%d" % (nm, sl), [128, 4, 256])
                B["am"] = sb(st3, "am_%d" % sl, [128, 128])
                B["ssq"] = sb(st3, "ssq_%d" % sl, [128, 1])
                B["rn1"] = sb(st3, "rn1_%d" % sl, [128, 1])
                B["res_"] = sb(st3, "res__%d" % sl, [128, 256])
                B["mgT"] = sb(st3, "mgT_%d" % sl, [128, 2, 512])
                B["junk"] = sb(st3, "junk3_%d" % sl, [128, 256])
                return B
            slots = [mkslot(0), mkslot(1)]

            def gla_gen(h, sl):
                B = slots[sl]
                K_ = lambda nm: "%s_%d" % (nm, sl)
                bq_, bk_, bm_, bo_ = 4 * sl, 4 * sl + 1, 4 * sl + 2, 4 * sl + 3
                wbf2, stt_ = B["wbf2"], B["st"]
                spl, cs, eb, enb, ed, dec = B["spl"], B["cs"], B["eb"], B["enb"], B["ed"], B["dec"]
                qt, kt, kd, kdt, vt, sog, sga = B["qt"], B["kt"], B["kd"], B["kdt"], B["vt"], B["sog"], B["sga"]
                am, ssq, rn1, res_, mgT, jk = B["am"], B["ssq"], B["rn1"], B["res_"], B["mgT"], B["junk"]
                cs3 = cs[:].rearrange("p (n c) -> p n c", c=64)
                P.dma(wbf2[:], wgla[h], writes=[K_("wbf2")], q="pool")
                cur = 0
                P.op("dve", lambda e: e.memset(stt_[0][:], 0.0), writes=[K_("st0")])
                for tb in range(NB):
                    ts_ = slice(tb * 512, (tb + 1) * 512)
                    uk = "uT%d" % tb
                    for which in range(2):
                        for kc in range(8):
                            P.op("pe", lambda e: e.matmul(pb[4 * sl + which][:], lhsT=wbf2[:, kc, which * 128:(which + 1) * 128],
                                                          rhs=uT[:, kc, ts_], start=(kc == 0), stop=(kc == 7)),
                                 reads=[K_("wbf2"), uk], writes=[pbk[4 * sl + which]])
                    P.op("pe", lambda e: e.matmul(pb[bm_][:], lhsT=wg2_sb[0:16, h * 128:(h + 1) * 128], rhs=glrT[0:16, ts_],
                                                  start=True, stop=True), reads=["wg2_sb", "glrT%d" % tb], writes=[pbk[bm_]])
                    yield
                    P.op("act", lambda e: e.activation(out=spl[:], in_=pb[bm_][:], func=AF.Exp, scale=-1.0, bias=negb[:, h:h + 1]),
                         reads=[pbk[bm_], "negb"], writes=[K_("spl")])
                    P.op("act", lambda e: e.activation(out=spl[:], in_=spl[:], func=AF.Ln, bias=1.0), reads=[K_("spl")], writes=[K_("spl")])
                    P.op("dve", lambda e: e.tensor_tensor_scan(out=cs[:], data0=cmask[:], data1=spl[:], initial=0.0,
                                                               op0=ALU.mult, op1=ALU.add), reads=["cmask", K_("spl")], writes=[K_("cs")])
                    yield
                    P.op("act", lambda e: e.activation(out=eb[:], in_=cs[:], func=AF.Exp, scale=-1.0 / 16), reads=[K_("cs")], writes=[K_("eb")])
                    P.op("act", lambda e: e.activation(out=enb[:], in_=cs[:], func=AF.Exp, scale=1.0 / 16), reads=[K_("cs")], writes=[K_("enb")])
                    P.op("dve", lambda e: e.tensor_tensor(out=ed[:].rearrange("p (n c) -> p n c", c=64), in0=cs3,
                                                          in1=cs3[:, :, 63:64].to_broadcast([128, 8, 64]), op=ALU.subtract),
                         reads=[K_("cs")], writes=[K_("ed")])
                    P.op("act", lambda e: e.activation(out=ed[:], in_=ed[:], func=AF.Exp, scale=1.0 / 16), reads=[K_("ed")], writes=[K_("ed")])
                    P.op("act", lambda e: e.activation(out=dec[:], in_=cs3[:, :, 63], func=AF.Exp, scale=-1.0 / 16),
                         reads=[K_("cs")], writes=[K_("dec")])
                    yield
                    P.op("dve", lambda e: e.scalar_tensor_tensor(out=qt[:], in0=pb[bq_][:], scalar=128.0 ** -0.5, in1=eb[:],
                                                                  op0=ALU.mult, op1=ALU.mult), reads=[pbk[bq_], K_("eb")], writes=[K_("qt")])
                    P.op("dve", lambda e: e.tensor_tensor(out=kt[:], in0=pb[bk_][:], in1=enb[:], op=ALU.mult),
                         reads=[pbk[bk_], K_("enb")], writes=[K_("kt")])
                    P.op("dve", lambda e: e.tensor_tensor(out=kd[:], in0=pb[bk_][:], in1=ed[:], op=ALU.mult),
                         reads=[pbk[bk_], K_("ed")], writes=[K_("kd")])
                    yield
                    for j in range(4):
                        P.op("pe", lambda e: e.transpose(pb[bm_][:, j * 128:(j + 1) * 128], kd[:, j * 128:(j + 1) * 128], ident[:]),
                             reads=[K_("kd"), "ident"], writes=[pbk[bm_]])
                    P.op("act", lambda e: e.copy(out=kdt[:], in_=pb[bm_][:].rearrange("p (j d) -> p j d", d=128)),
                         reads=[pbk[bm_]], writes=[K_("kdt")])
                    yield
                    for j in range(4):
                        tj = slice(tb * 512 + j * 128, tb * 512 + (j + 1) * 128)
                        for kc in range(8):
                            P.op("pe", lambda e: e.matmul(pb[bq_][:], lhsT=uT[:, kc, tj], rhs=wbf2[:, kc, 256:768],
                                                          start=(kc == 0), stop=(kc == 7)), reads=[K_("wbf2"), uk], writes=[pbk[bq_]])
                        for kc in range(8):
                            P.op("pe", lambda e: e.matmul(pb[bk_][:, 0:256], lhsT=uT[:, kc, tj], rhs=wbf2[:, kc, 768:1024],
                                                          start=(kc == 0), stop=(kc == 7)), reads=[K_("wbf2"), uk], writes=[pbk[bk_]])
                        P.op("act", lambda e: e.copy(out=vt[:, j, :], in_=pb[bq_][:, 0:256]), reads=[pbk[bq_]], writes=[K_("vt%d" % j)])
                        P.op("act", lambda e: e.activation(out=sog[:, j, :], in_=pb[bq_][:, 256:512], func=AF.Silu),
                             reads=[pbk[bq_]], writes=[K_("sog%d" % j)])
                        P.op("act", lambda e: e.activation(out=sga[:, j, :], in_=pb[bk_][:, 0:256], func=AF.Sigmoid),
                             reads=[pbk[bk_]], writes=[K_("sga%d" % j)])
                        yield
                    for j in range(4):
                        js = slice(j * 128, (j + 1) * 128)
                        P.op("pe", lambda e: e.matmul(pb[bm_][:, 256:384], lhsT=kt[:, js], rhs=qt[:, js], start=True, stop=True),
                             reads=[K_("kt"), K_("qt")], writes=[pbk[bm_]])
                        P.op("dve", lambda e: e.tensor_tensor(out=am[:], in0=pb[bm_][:, 256:384], in1=mask2[:], op=ALU.mult),
                             reads=[pbk[bm_], "mask2"], writes=[K_("am")])
                        yield
                        P.op("pe", lambda e: e.matmul(pb[bo_][:, 0:256], lhsT=am[:], rhs=vt[:, j, :], start=True, stop=False),
                             reads=[K_("am"), K_("vt%d" % j)], writes=[pbk[bo_]])
                        for half in range(2):
                            n = 2 * j + half
                            hs = slice(half * 64, (half + 1) * 64)
                            P.op("pe", lambda e: e.matmul(pb[bo_][hs, 0:256], lhsT=qt[:, n * 64:(n + 1) * 64], rhs=stt_[cur][:],
                                                          start=False, stop=(half == 1)),
                                 reads=[K_("qt"), K_("st%d" % cur)], writes=[pbk[bo_]])
                            if half == 0:
                                P.op("pe", lambda e: e.matmul(pb[bm_][:, 0:256], lhsT=kdt[hs, j, :], rhs=vt[hs, j, :], start=True, stop=True),
                                     reads=[K_("kdt"), K_("vt%d" % j)], writes=[pbk[bm_]])
                                P.op("dve", lambda e: e.scalar_tensor_tensor(out=stt_[1 - cur][:], in0=stt_[cur][:], scalar=dec[:, n:n + 1],
                                                                              in1=pb[bm_][:, 0:256], op0=ALU.mult, op1=ALU.add),
                                     reads=[K_("st%d" % cur), K_("dec"), pbk[bm_]], writes=[K_("st%d" % (1 - cur))])
                                cur = 1 - cur
                                yield
                        P.op("act", lambda e: e.activation(out=jk[:], in_=pb[bo_][:, 0:256], func=AF.Square, accum_out=ssq[:]),
                             reads=[pbk[bo_]], writes=[K_("junk"), K_("ssq")])
                        P.op("act", lambda e: e.activation(out=rn1[:], in_=ssq[:], func=AF.Sqrt, scale=1.0 / 256, bias=RMS_EPS),
                             reads=[K_("ssq")], writes=[K_("rn1")])
                        P.op("dve", lambda e: e.reciprocal(out=rn1[:], in_=rn1[:]), reads=[K_("rn1")], writes=[K_("rn1")])
                        P.op("dve", lambda e: e.scalar_tensor_tensor(out=res_[:], in0=pb[bo_][:, 0:256], scalar=rn1[:, 0:1], in1=glanw_sb[:],
                                                                      op0=ALU.mult, op1=ALU.mult),
                             reads=[pbk[bo_], K_("rn1"), "glanw_sb"], writes=[K_("res_")])
                        hs = slice(64, 128)
                        n = 2 * j + 1
                        P.op("pe", lambda e: e.matmul(pb[bm_][:, 0:256], lhsT=kdt[hs, j, :], rhs=vt[hs, j, :], start=True, stop=True),
                             reads=[K_("kdt"), K_("vt%d" % j)], writes=[pbk[bm_]])
                        P.op("dve", lambda e: e.scalar_tensor_tensor(out=stt_[1 - cur][:], in0=stt_[cur][:], scalar=dec[:, n:n + 1],
                                                                      in1=pb[bm_][:, 0:256], op0=ALU.mult, op1=ALU.add),
                             reads=[K_("st%d" % cur), K_("dec"), pbk[bm_]], writes=[K_("st%d" % (1 - cur))])
                        cur = 1 - cur
                        yield
                        P.op("dve", lambda e: e.tensor_tensor(out=res_[:], in0=res_[:], in1=sog[:, j, :], op=ALU.mult),
                             reads=[K_("res_"), K_("sog%d" % j)], writes=[K_("res_")])
                        P.op("dve", lambda e: e.tensor_tensor(out=res_[:], in0=res_[:], in1=sga[:, j, :], op=ALU.mult),
                             reads=[K_("res_"), K_("sga%d" % j)], writes=[K_("res_")])
                        for f in range(2):
                            P.op("pe", lambda e: e.transpose(pb[bk_][:, 256 + f * 128:256 + (f + 1) * 128],
                                                             res_[:, f * 128:(f + 1) * 128], ident[:]),
                                 reads=[K_("res_"), "ident"], writes=[pbk[bk_]])
                        P.op("act", lambda e: e.copy(out=mgT[:, :, js], in_=pb[bk_][:, 256:512].rearrange("p (f t) -> p f t", t=128)),
                             reads=[pbk[bk_]], writes=[K_("mgT")])
                        yield
                    P.dma(mixg[h * 256:(h + 1) * 256, ts_].rearrange("(f p) t -> p f t", p=128), mgT[:],
                          reads=[K_("mgT")], writes=["mixg"])
                    yield

            def interleave3(gens):
                gens = list(gens)
                while gens:
                    for g in list(gens):
                        try:
                            next(g)
                        except StopIteration:
                            gens.remove(g)
            for hp in range(2):
                interleave3([gla_gen(2 * hp, 0), gla_gen(2 * hp + 1, 1)])

    _sc.__exit__(None, None, None)
    if dbg == 1 and stage == 3:
        with ExitStack() as sd:
            tmp = sb(sd, "dbg_tmp3", [128, 8, 512])
            for tb in range(NB):
                P.dma(tmp[:], fm(mixg)[:, :, tb * 512:(tb + 1) * 512], reads=["mixg"], writes=["dbg_tmp3"])
                P.dma(fm(outT)[:, :, tb * 512:(tb + 1) * 512], tmp[:], reads=["dbg_tmp3"], writes=["outT"])


    P.barrier()
    mix_stack.close()
    _sc = nc.named_scope('st4'); _sc.__enter__()
    if stage >= 4:
        with ExitStack() as st4:
            wout_r = sb(st4, "wout_r", [128, 8, 1024], F32R)
            wr_sb = sb(st4, "wr_sb", [128, 8, 36])
            br_sb = sb(st4, "br_sb", [1, 36])
            mdb = sb(st4, "mdb", [128, 8, 512])
            mgb = sb(st4, "mgb", [128, 8, 512])
            mixr = sb(st4, "mixr", [128, 8, 512], F32R)
            xt4 = sb(st4, "xt4", [128, 8, 512])
            x1b = sb(st4, "x1b", [128, 8, 512])
            sq = sb(st4, "sq4", [128, 8, 512], F32R)
            mean = sb(st4, "mean4", [128, 512])
            rstd = sb(st4, "rstd4", [128, 512])
            L = sb(st4, "L", [128, 4, 36])
            r4 = [sb(st4, "r4_%d" % i, [128, 4]) for i in range(8)]
            g44 = sb(st4, "g44", [128, 4, 4])
            lem = sb(st4, "lem", [128, 4, 32])
            lem2 = sb(st4, "lem2", [128, 4, 32])
            m1 = sb(st4, "m1", [128, 4, 32])
            m2 = sb(st4, "m2", [128, 4, 32])
            P.dma(wout_r[:], wout, writes=["wout_r"], q="pool")
            P.dma(wr_sb[:], wr, writes=["wr_sb"])
            P.dma(br_sb[:], br, writes=["br_sb"])

            def bc4(t, n):
                return t[:].unsqueeze(2).to_broadcast([128, 4, n])

            for tb in range(NB):
                ts_ = slice(tb * 512, (tb + 1) * 512)
                P.dma(mdb[:], fm(mixd)[:, :, ts_], reads=["mixd"], writes=["mdb"])
                P.dma(mgb[:], fm(mixg)[:, :, ts_], reads=["mixg"], writes=["mgb"])
                P.dma(xt4[:], fm(xT)[:, :, ts_], writes=["xt4"])
                P.op("dve", lambda e: e.tensor_tensor(out=mixr[:], in0=mdb[:], in1=mgb[:], op=ALU.add),
                     reads=["mdb", "mgb"], writes=["mixr"])
                P.op("act", lambda e: e.mul(out=xt4[:], in_=xt4[:], mul=ALPHA), reads=["xt4"], writes=["xt4"])
                for dc in range(8):
                    bank = 2 + dc % 2
                    for kc in range(8):
                        P.op("pe", lambda e: e.matmul(pb[bank][:], lhsT=wout_r[:, kc, dc * 128:(dc + 1) * 128], rhs=mixr[:, kc, :],
                                                      start=(kc == 0), stop=(kc == 7)), reads=["wout_r", "mixr"], writes=[pbk[bank]])
                    P.op("dve", lambda e: e.scalar_tensor_tensor(out=xt4[:, dc, :], in0=pb[bank][:], scalar=adaT[:, G1 + dc:G1 + dc + 1],
                                                                  in1=xt4[:, dc, :], op0=ALU.mult, op1=ALU.add),
                         reads=[pbk[bank], "adaT", "xt4"], writes=["xt4"])
                ln_block(xt4[:], "xt4", mean, rstd, sq, "4")
                ln_apply(xt4[:], "xt4", xt4[:], "xt4", mean, rstd, "4")
                for dc in range(8):
                    P.op("act", lambda e: e.activation(out=x1b[:, dc, :], in_=xt4[:, dc, :], func=AF.Identity,
                                                       scale=lnp_sb[:, 0, dc:dc + 1], bias=lnp_sb[:, 1, dc:dc + 1]),
                         reads=["xt4", "lnp_sb"], writes=["x1b"])
                P.dma(fm(x1T)[:, :, ts_], x1b[:], reads=["x1b"], writes=["x1T"])
                ln_block(x1b[:], "x1b", mean, rstd, sq, "4")
                ln_apply(mdb[:], "mdb", x1b[:], "x1b", mean, rstd, "4")
                for dc in range(8):
                    P.op("act", lambda e: e.activation(out=mgb[:, dc, :], in_=mdb[:, dc, :], func=AF.Identity,
                                                       scale=s2p[:, dc:dc + 1], bias=adaT[:, SH2 + dc:SH2 + dc + 1]),
                         reads=["mdb", "s2p", "adaT"], writes=["mgb"])
                P.dma(fm(u2T)[:, :, ts_], mgb[:], reads=["mgb"], writes=["u2T"])
                for j in range(4):
                    for kc in range(8):
                        P.op("pe", lambda e: e.matmul(pb[4][:, j * 36:(j + 1) * 36], lhsT=mgb[:, kc, j * 128:(j + 1) * 128],
                                                      rhs=wr_sb[:, kc, :], start=(kc == 0), stop=False),
                             reads=["mgb", "wr_sb"], writes=[pbk[4]])
                    P.op("pe", lambda e: e.matmul(pb[4][:, j * 36:(j + 1) * 36], lhsT=ones[0:1, :], rhs=br_sb[0:1, :],
                                                  start=False, stop=True), reads=["ones", "br_sb"], writes=[pbk[4]])
                P.op("dve", lambda e: e.tensor_copy(out=L[:], in_=pb[4][:, 0:144].rearrange("p (j n) -> p j n", n=36)),
                     reads=[pbk[4]], writes=["L"])
                lg_ = L[:, :, 0:4]
                le_ = L[:, :, 4:36]
                gmax, gsum, wgrp, v1, v2, ex, w1, w2 = r4
                P.op("dve", lambda e: e.tensor_reduce(out=gmax[:], in_=lg_, axis=AX.X, op=ALU.max), reads=["L"], writes=["gmax"])
                P.op("dve", lambda e: e.tensor_tensor(out=g44[:], in0=lg_, in1=bc4(gmax, 4), op=ALU.subtract),
                     reads=["L", "gmax"], writes=["g44"])
                P.op("act", lambda e: e.activation(out=g44[:], in_=g44[:], func=AF.Exp), reads=["g44"], writes=["g44"])
                P.op("dve", lambda e: e.tensor_reduce(out=gsum[:], in_=g44[:], axis=AX.X, op=ALU.add), reads=["g44"], writes=["gsum"])
                P.op("dve", lambda e: e.reciprocal(out=wgrp[:], in_=gsum[:]), reads=["gsum"], writes=["wgrp"])
                P.op("dve", lambda e: e.tensor_tensor(out=g44[:], in0=lg_, in1=bc4(gmax, 4), op=ALU.is_equal),
                     reads=["L", "gmax", "g44"], writes=["g44"])
                P.op("dve", lambda e: e.tensor_scalar(out=g44[:], in0=g44[:], scalar1=-1.0, scalar2=BIG, op0=ALU.add, op1=ALU.mult),
                     reads=["g44"], writes=["g44"])
                P.op("dve", lambda e: e.tensor_tensor(out=lem[:].rearrange("p j (g k) -> p j g k", k=8),
                                                      in0=le_.rearrange("p j (g k) -> p j g k", k=8),
                                                      in1=g44[:].unsqueeze(3).to_broadcast([128, 4, 4, 8]), op=ALU.add),
                     reads=["L", "g44"], writes=["lem"])
                P.op("dve", lambda e: e.tensor_reduce(out=v1[:], in_=lem[:], axis=AX.X, op=ALU.max), reads=["lem"], writes=["v1"])
                P.op("dve", lambda e: e.tensor_tensor(out=m1[:], in0=lem[:], in1=bc4(v1, 32), op=ALU.is_equal),
                     reads=["lem", "v1"], writes=["m1"])
                P.op("dve", lambda e: e.scalar_tensor_tensor(out=lem2[:], in0=m1[:], scalar=-BIG, in1=lem[:], op0=ALU.mult, op1=ALU.add),
                     reads=["m1", "lem"], writes=["lem2"])
                P.op("dve", lambda e: e.tensor_reduce(out=v2[:], in_=lem2[:], axis=AX.X, op=ALU.max), reads=["lem2"], writes=["v2"])
                P.op("dve", lambda e: e.tensor_tensor(out=m2[:], in0=lem2[:], in1=bc4(v2, 32), op=ALU.is_equal),
                     reads=["lem2", "v2"], writes=["m2"])
                P.op("dve", lambda e: e.tensor_tensor(out=ex[:], in0=v2[:], in1=v1[:], op=ALU.subtract), reads=["v1", "v2"], writes=["ex"])
                P.op("act", lambda e: e.activation(out=ex[:], in_=ex[:], func=AF.Exp), reads=["ex"], writes=["ex"])
                P.op("dve", lambda e: e.tensor_scalar(out=w1[:], in0=ex[:], scalar1=1.0, scalar2=None, op0=ALU.add), reads=["ex"], writes=["w1"])
                P.op("dve", lambda e: e.reciprocal(out=w1[:], in_=w1[:]), reads=["w1"], writes=["w1"])
                P.op("dve", lambda e: e.tensor_tensor(out=w2[:], in0=ex[:], in1=w1[:], op=ALU.mult), reads=["ex", "w1"], writes=["w2"])
                P.op("dve", lambda e: e.tensor_tensor(out=w1[:], in0=w1[:], in1=wgrp[:], op=ALU.mult), reads=["w1", "wgrp"], writes=["w1"])
                P.op("dve", lambda e: e.tensor_tensor(out=w2[:], in0=w2[:], in1=wgrp[:], op=ALU.mult), reads=["w2", "wgrp"], writes=["w2"])
                P.op("dve", lambda e: e.tensor_tensor(out=m1[:], in0=m1[:], in1=bc4(w1, 32), op=ALU.mult), reads=["m1", "w1"], writes=["m1"])
                P.op("dve", lambda e: e.tensor_tensor(out=m2[:], in0=m2[:], in1=bc4(w2, 32), op=ALU.mult), reads=["m2", "w2"], writes=["m2"])
                P.op("dve", lambda e: e.tensor_tensor(out=Wts[:, tb * 4:(tb + 1) * 4, :], in0=m1[:], in1=m2[:], op=ALU.add),
                     reads=["m1", "m2"], writes=["Wts"])

    _sc.__exit__(None, None, None)
    if dbg == 1 and stage == 4:
        with ExitStack() as sd:
            tmp = sb(sd, "dbg_tmp4", [128, 8, 512])
            for tb in range(NB):
                P.dma(tmp[:], fm(u2T)[:, :, tb * 512:(tb + 1) * 512], reads=["u2T"], writes=["dbg_tmp4"])
                P.dma(fm(outT)[:, :, tb * 512:(tb + 1) * 512], tmp[:], reads=["dbg_tmp4"], writes=["outT"])

    P.barrier()
    _sc = nc.named_scope('st5'); _sc.__enter__()
    if stage >= 5:
        NTQ = TQ // 128
        NBQ = TQ // 512
        for q in range(NQ):
            with ExitStack() as sq5:
                qs = slice(q * TQ, (q + 1) * TQ)
                u2r = sb(sq5, "u2r", [128, 8, TQ], F32R)
                yacc = sb(sq5, "yacc", [128, NTQ, 1024])
                P.dma(u2r[:], fm(u2T)[:, :, qs], reads=["u2T"], writes=["u2r"], q="pool")
                with ExitStack() as se5:
                    gur = Ring([sb(se5, "wgu%d" % i, [128, 8, 1024], F32R) for i in range(2)], "wgu")
                    wdr = Ring([sb(se5, "wd%d" % i, [128, 4, 1024], F32R) for i in range(2)], "wd")
                    hring = Ring([sb(se5, "hT%d" % i, [128, 4, 512], F32R) for i in range(2)], "hT")
                    sgr = Ring([sb(se5, "sg%d" % i, [128, 512]) for i in range(2)], "sg")
                    cnt5 = {"it": 0, "yi": 0}
                    wcur = {}

                    def emit_gu(ex_, bq):
                        if bq == 0:
                            wg_, wgk = gur.next()
                            wd_, wdk = wdr.next()
                            P.dma(wg_[:], wgu[ex_], writes=[wgk], q="pool")
                            P.dma(wd_[:], wd[ex_], writes=[wdk], q="pool")
                            wcur[ex_] = (wg_, wgk, wd_, wdk)
                        wg_, wgk, wd_, wdk = wcur[ex_]
                        bs = slice(bq * 512, (bq + 1) * 512)
                        hT, hk = hring.next()
                        for fc in range(4):
                            it = cnt5["it"]
                            cnt5["it"] += 1
                            bg, bu = (it % 2) * 2, (it % 2) * 2 + 1
                            for kc in range(8):
                                P.op("pe", lambda e: e.matmul(pb[bg][:], lhsT=wg_[:, kc, fc * 128:(fc + 1) * 128], rhs=u2r[:, kc, bs],
                                                              start=(kc == 0), stop=(kc == 7)), reads=[wgk, "u2r"], writes=[pbk[bg]])
                            for kc in range(8):
                                P.op("pe", lambda e: e.matmul(pb[bu][:], lhsT=wg_[:, kc, 512 + fc * 128:512 + (fc + 1) * 128],
                                                              rhs=u2r[:, kc, bs], start=(kc == 0), stop=(kc == 7)),
                                     reads=[wgk, "u2r"], writes=[pbk[bu]])
                            sg_, sgk = sgr.next()
                            P.op("act", lambda e: e.activation(out=sg_[:], in_=pb[bg][:], func=AF.Silu), reads=[pbk[bg]], writes=[sgk])
                            P.op("dve", lambda e: e.tensor_tensor(out=hT[:, fc, :], in0=sg_[:], in1=pb[bu][:], op=ALU.mult),
                                 reads=[sgk, pbk[bu]], writes=[hk])
                        return hT, hk

                    def emit_y(ex_, bq, hT, hk):
                        wg_, wgk, wd_, wdk = wcur[ex_]
                        for tt in range(4):
                            tile = bq * 4 + tt
                            gt = q * NTQ + tile
                            for dh in range(2):
                                by = 4 + cnt5["yi"] % 2
                                cnt5["yi"] += 1
                                for fc in range(4):
                                    P.op("pe", lambda e: e.matmul(pb[by][:], lhsT=hT[:, fc, tt * 128:(tt + 1) * 128],
                                                                  rhs=wd_[:, fc, dh * 512:(dh + 1) * 512],
                                                                  start=(fc == 0), stop=(fc == 3)), reads=[hk, wdk], writes=[pbk[by]])
                                ya = yacc[:, tile, dh * 512:(dh + 1) * 512]
                                yk = "yacc%d_%d" % (tile, dh)
                                if ex_ == 0:
                                    P.op("dve", lambda e: e.tensor_scalar(out=ya, in0=pb[by][:], scalar1=Wts[:, gt, ex_:ex_ + 1],
                                                                          scalar2=None, op0=ALU.mult),
                                         reads=[pbk[by], "Wts"], writes=[yk])
                                else:
                                    P.op("dve", lambda e: e.scalar_tensor_tensor(out=ya, in0=pb[by][:], scalar=Wts[:, gt, ex_:ex_ + 1],
                                                                                  in1=ya, op0=ALU.mult, op1=ALU.add),
                                         reads=[pbk[by], "Wts", yk], writes=[yk])

                    blocks = [(ex_, bq) for ex_ in range(32) for bq in range(NBQ)]
                    if PIPE_MOE:
                        pend = emit_gu(*blocks[0])
                        for bi, (ex_, bq) in enumerate(blocks):
                            nxt = emit_gu(*blocks[bi + 1]) if bi + 1 < len(blocks) else None
                            emit_y(ex_, bq, *pend)
                            pend = nxt
                    else:
                        for (ex_, bq) in blocks:
                            emit_y(ex_, bq, *emit_gu(ex_, bq))
                P.barrier()
                with ExitStack() as sf5:
                    mT = sb(sf5, "mT", [128, 8, 512])
                    x1a = sb(sf5, "x1a", [128, 8, 512])
                    sq = sb(sf5, "sq5", [128, 8, 512], F32R)
                    mean = sb(sf5, "mean5", [128, 512])
                    rstd = sb(sf5, "rstd5", [128, 512])
                    for bq in range(NBQ):
                        gs = slice(q * TQ + bq * 512, q * TQ + (bq + 1) * 512)
                        P.dma(x1a[:], fm(x1T)[:, :, gs], reads=["x1T"], writes=["x1a"])
                        P.op("act", lambda e: e.mul(out=x1a[:], in_=x1a[:], mul=ALPHA), reads=["x1a"], writes=["x1a"])
                        for tt in range(4):
                            tile = bq * 4 + tt
                            for g4 in range(2):
                                bank = 6 + g4
                                for d4 in range(4):
                                    dc = g4 * 4 + d4
                                    P.op("pe", lambda e: e.transpose(pb[bank][:, d4 * 128:(d4 + 1) * 128],
                                                                     yacc[:, tile, dc * 128:(dc + 1) * 128], ident[:]),
                                         reads=["yacc%d_%d" % (tile, dc // 4), "ident"], writes=[pbk[bank]])
                                P.op("act", lambda e: e.copy(out=mT[:, g4 * 4:(g4 + 1) * 4, tt * 128:(tt + 1) * 128],
                                                             in_=pb[bank][:].rearrange("p (d t) -> p d t", t=128)),
                                     reads=[pbk[bank]], writes=["mT"])
                        for dc in range(8):
                            P.op("dve", lambda e: e.scalar_tensor_tensor(out=mT[:, dc, :], in0=mT[:, dc, :], scalar=adaT[:, G2 + dc:G2 + dc + 1],
                                                                          in1=x1a[:, dc, :], op0=ALU.mult, op1=ALU.add),
                                 reads=["mT", "adaT", "x1a"], writes=["mT"])
                        ln_block(mT[:], "mT", mean, rstd, sq, "5")
                        ln_apply(mT[:], "mT", mT[:], "mT", mean, rstd, "5")
                        for dc in range(8):
                            P.op("act", lambda e: e.activation(out=x1a[:, dc, :], in_=mT[:, dc, :], func=AF.Identity,
                                                               scale=lnp_sb[:, 2, dc:dc + 1], bias=lnp_sb[:, 3, dc:dc + 1]),
                                 reads=["mT", "lnp_sb"], writes=["x1a"])
                        P.dma(fm(outT)[:, :, gs], x1a[:], reads=["x1a"], writes=["outT"])
            P.barrier()

    _sc.__exit__(None, None, None)
    P.wait_all("sp", ["outT"])
    root.close()
    P.close()
    return nc, P


def _host_inputs(inp, S):
    f = np.float32
    B = inp["x"].shape[0]

    def pc(w):
        return np.ascontiguousarray(w.reshape(8, 128, -1).transpose(1, 0, 2))

    def col8(v):
        return np.ascontiguousarray(v.reshape(8, 128).T)

    w_in = inp["w_in"][0]
    gq, gk, gv, go, gr = w_in[:, 0:512], w_in[:, 512:1024], w_in[:, 1024:2048], w_in[:, 2048:3072], w_in[:, 3072:3088]
    dq, dk, dv = w_in[:, 3088:4112], w_in[:, 4112:5136], w_in[:, 5136:6160]
    ga, gb = w_in[:, 6160:7184], w_in[:, 7184:8208]
    wdiff = np.stack([pc(np.concatenate([dq[:, h * 128:(h + 1) * 128], dk[:, h * 128:(h + 1) * 128],
                                         dv[:, h * 128:(h + 1) * 128], gb[:, h * 128:(h + 1) * 128]], axis=1))
                      for h in range(8)])
    wgla = np.stack([pc(np.concatenate([gq[:, h * 128:(h + 1) * 128], gk[:, h * 128:(h + 1) * 128],
                                        gv[:, h * 256:(h + 1) * 256], go[:, h * 256:(h + 1) * 256],
                                        ga[:, h * 256:(h + 1) * 256]], axis=1)) for h in range(4)])
    inv = (10000.0 ** (-np.arange(32, dtype=f) / 32)).astype(f)
    p = np.arange(128)
    shared = {
        "invf": np.ascontiguousarray(inv[p % 32].reshape(128, 1)),
        "sgn": np.where((p % 64) < 32, -1.0, 1.0).astype(f).reshape(128, 1),
        "wada": pc(inp["w_ada"][0]),
        "bada": np.ascontiguousarray(inp["b_ada"][0].reshape(1, 6144)),
        "wdiff": np.ascontiguousarray(wdiff),
        "wgla": np.ascontiguousarray(wgla),
        "wgr": pc(gr),
        "wg2": np.ascontiguousarray(inp["w_gla_gate2"][0]),
        "bg2c": np.ascontiguousarray(inp["b_gla_gate2"][0].reshape(4, 128).T),
        "glanw": np.ascontiguousarray(np.broadcast_to(inp["gla_norm_w"][0][None, :], (128, 256))),
        "dnw": np.ascontiguousarray(inp["diff_norm_w"][0].reshape(128, 1)),
        "lamv": np.ascontiguousarray(np.broadcast_to(np.stack([inp["diff_lambda_q1"][0], inp["diff_lambda_k1"][0],
                                                               inp["diff_lambda_q2"][0], inp["diff_lambda_k2"][0]])[None],
                                                     (128, 4, 64))),
        "wout": pc(inp["w_out"][0]),
        "lnp": np.ascontiguousarray(np.stack([col8(inp["ln1_w"][0]), col8(inp["ln1_b"][0]),
                                              col8(inp["ln2_w"][0]), col8(inp["ln2_b"][0])], axis=1)),
        "wr": pc(np.concatenate([inp["w_router_group"][0], inp["w_router_expert"][0]], axis=1)),
        "br": np.ascontiguousarray(np.concatenate([inp["b_router_group"][0], inp["b_router_expert"][0]]).reshape(1, 36)),
        "wgu": np.ascontiguousarray(np.concatenate([inp["w_exp_gate"][0], inp["w_exp_up"][0]], axis=2)
                                    .reshape(32, 8, 128, 1024).transpose(0, 2, 1, 3)),
        "wd": np.ascontiguousarray(inp["w_exp_down"][0].reshape(32, 4, 128, 1024).transpose(0, 2, 1, 3)),
    }
    shared = {k: np.asarray(v, dtype=f) for k, v in shared.items()}
    maps = []
    for b in range(B):
        m = dict(shared)
        m["xT"] = np.ascontiguousarray(inp["x"][b, :S].T.astype(f))
        m["ccol"] = np.ascontiguousarray(inp["c"][b].reshape(8, 128).T.astype(f))
        m["posb"] = np.ascontiguousarray(np.broadcast_to(inp["positions"][b, :S].astype(np.int32)[None, :], (128, S)))
        maps.append(m)
    return maps


def kernel(**inputs):
    inp = {k: np.asarray(v) for k, v in inputs.items()}
    B, S, _ = inp["x"].shape
    nc, _ = build_program(S)
    maps = _host_inputs(inp, S)
    res = run_bass_kernel_spmd(nc, maps, core_ids=list(range(B)))
    out = np.stack([np.ascontiguousarray(res.results[b]["outT"].T) for b in range(B)])
    return out.astype(np.float32)
```
